# Optimizing a Trainium2 kernel written in Bass

```python
import math
import jax
import jax.numpy as jnp
from jax import lax
import numpy as np

D_MODEL = 2048
BATCH = 4
SEQ = 4096
DEPTH = 2

N_MIXERS = 2
N_A = (DEPTH + 1) // 2
N_B = DEPTH // 2
RMS_EPS = 1e-6

MLA_HEADS = 16
MLA_Q_RANK = 512
MLA_KV_RANK = 512
MLA_NOPE = 128
MLA_ROPE = 64
MLA_V = 128
MLA_IN_DIM = MLA_Q_RANK + MLA_KV_RANK + MLA_ROPE
ROPE_THETA = 10000.0
Q_BLOCK = 128

SSD_INNER = 2 * D_MODEL
SSD_HEADDIM = 64
SSD_HEADS = SSD_INNER // SSD_HEADDIM
SSD_GROUPS = 8
SSD_STATE = 128
SSD_CONV = 4
SSD_CHUNK = 256
SSD_GN = SSD_GROUPS * SSD_STATE
SSD_CONV_DIM = SSD_INNER + 2 * SSD_GN
SSD_IN_DIM = 2 * SSD_INNER + 2 * SSD_GN + SSD_HEADS

FFN_DIM = 7168
N_EXPERTS = 8
TOP_K = 2
MOE_BLOCK = 512

kernel_name = "hybrid_mla_ssd_moe_trunk"


def rmsnorm(x, g):
    xf = x.astype(jnp.float32)
    r = lax.rsqrt(jnp.mean(xf * xf, axis=-1, keepdims=True) + RMS_EPS)
    return (xf * r).astype(x.dtype) * g


def apply_rope(t, cos, sin):
    half = t.shape[-1] // 2
    t1, t2 = t[..., :half], t[..., half:]
    return jnp.concatenate([t1 * cos - t2 * sin, t1 * sin + t2 * cos], axis=-1)


def mla_mixer(h, positions, w_in, q_norm, kv_norm, w_uq, w_ukv, w_o):
    bsz, s, _ = h.shape
    c = h @ w_in
    c_q = c[..., :MLA_Q_RANK]
    c_kv = c[..., MLA_Q_RANK:MLA_Q_RANK + MLA_KV_RANK]
    k_rope = c[..., MLA_Q_RANK + MLA_KV_RANK:]
    q = (rmsnorm(c_q, q_norm) @ w_uq).reshape(bsz, s, MLA_HEADS, MLA_NOPE + MLA_ROPE)
    q_nope, q_rope = q[..., :MLA_NOPE], q[..., MLA_NOPE:]
    kv = (rmsnorm(c_kv, kv_norm) @ w_ukv).reshape(bsz, s, MLA_HEADS, MLA_NOPE + MLA_V)
    k_nope, v = kv[..., :MLA_NOPE], kv[..., MLA_NOPE:]
    inv_freq = ROPE_THETA ** (-jnp.arange(0, MLA_ROPE, 2, dtype=jnp.float32) / MLA_ROPE)
    ang = positions.astype(jnp.float32)[..., None] * inv_freq
    cos, sin = jnp.cos(ang).astype(h.dtype), jnp.sin(ang).astype(h.dtype)
    q_rope = apply_rope(q_rope, cos[:, :, None], sin[:, :, None])
    k_rope = apply_rope(k_rope, cos, sin)
    scale = (MLA_NOPE + MLA_ROPE) ** -0.5
    nq = s // Q_BLOCK
    qn_b = q_nope.reshape(bsz, nq, Q_BLOCK, MLA_HEADS, MLA_NOPE).swapaxes(0, 1)
    qr_b = q_rope.reshape(bsz, nq, Q_BLOCK, MLA_HEADS, MLA_ROPE).swapaxes(0, 1)
    key_pos = jnp.arange(s)

    def attend(args):
        qn, qr, qi = args
        sc = jnp.einsum('bqhd,bkhd->bhqk', qn, k_nope) + jnp.einsum('bqhr,bkr->bhqk', qr, k_rope)
        sc = sc.astype(jnp.float32) * scale
        q_pos = qi * Q_BLOCK + jnp.arange(Q_BLOCK)
        sc = jnp.where(key_pos[None, :] <= q_pos[:, None], sc, -jnp.inf)
        p = jax.nn.softmax(sc, axis=-1).astype(v.dtype)
        return jnp.einsum('bhqk,bkhd->bqhd', p, v)

    o = lax.map(attend, (qn_b, qr_b, jnp.arange(nq)))
    o = o.swapaxes(0, 1).reshape(bsz, s, MLA_HEADS * MLA_V)
    return o @ w_o


def causal_dwconv(u, w, b):
    k = w.shape[0]
    out = lax.conv_general_dilated(u, w[:, None, :], window_strides=(1,), padding=[(k - 1, 0)],
                                   dimension_numbers=('NWC', 'WIO', 'NWC'),
                                   feature_group_count=u.shape[-1])
    return out + b


def ssd_chunked_scan(x, dt, a_neg, bm, cm):
    bsz, s, _, p = x.shape
    hg = SSD_HEADS // SSD_GROUPS
    L = SSD_CHUNK
    pad = (-s) % L
    if pad:
        padf = lambda t: jnp.pad(t, [(0, 0), (0, pad)] + [(0, 0)] * (t.ndim - 2))
        x, dt, bm, cm = padf(x), padf(dt), padf(bm), padf(cm)
    nc = (s + pad) // L
    to_chunks = lambda t: t.reshape((bsz, nc, L) + t.shape[2:]).swapaxes(0, 1)
    xc = to_chunks(x.reshape(bsz, s + pad, SSD_GROUPS, hg, p))
    dtc = to_chunks(dt.reshape(bsz, s + pad, SSD_GROUPS, hg))
    ac = dtc * a_neg.reshape(SSD_GROUPS, hg)
    bc, cc = to_chunks(bm), to_chunks(cm)
    causal = jnp.tril(jnp.ones((L, L), dtype=bool))

    def step(state, inp):
        xk, dtk, ak, bk, ck = inp
        acum = jnp.cumsum(ak, axis=1)
        seg = acum[:, :, None] - acum[:, None, :]
        decay = jnp.exp(jnp.where(causal[None, :, :, None, None], seg, -jnp.inf))
        cb = jnp.einsum('blgn,bsgn->blsg', ck, bk)
        xdt = xk * dtk[..., None]
        y_intra = jnp.einsum('blsg,blsgh,bsghp->blghp', cb, decay, xdt)
        y_state = jnp.einsum('blgn,bghpn->blghp', ck, state) * jnp.exp(acum)[..., None]
        to_end = jnp.exp(acum[:, -1:] - acum)
        new_state = state * jnp.exp(acum[:, -1])[..., None, None] + \
            jnp.einsum('bsgh,bsghp,bsgn->bghpn', to_end, xdt, bk)
        return new_state, y_intra + y_state

    state0 = jnp.zeros((bsz, SSD_GROUPS, hg, p, SSD_STATE), jnp.float32)
    _, ys = lax.scan(step, state0, (xc, dtc, ac, bc, cc))
    return ys.swapaxes(0, 1).reshape(bsz, nc * L, SSD_HEADS, p)[:, :s]


def ssd_mixer(h, w_in, conv_w, conv_b, dt_bias, a_log, d_skip, norm_g, w_o):
    bsz, s, _ = h.shape
    zxbcdt = h @ w_in
    z = zxbcdt[..., :SSD_INNER]
    xbc = zxbcdt[..., SSD_INNER:SSD_INNER + SSD_CONV_DIM]
    dt_raw = zxbcdt[..., SSD_INNER + SSD_CONV_DIM:]
    xbc = jax.nn.silu(causal_dwconv(xbc, conv_w, conv_b))
    xs = xbc[..., :SSD_INNER].reshape(bsz, s, SSD_HEADS, SSD_HEADDIM).astype(jnp.float32)
    bm = xbc[..., SSD_INNER:SSD_INNER + SSD_GN].reshape(bsz, s, SSD_GROUPS, SSD_STATE).astype(jnp.float32)
    cm = xbc[..., SSD_INNER + SSD_GN:].reshape(bsz, s, SSD_GROUPS, SSD_STATE).astype(jnp.float32)
    dt = jax.nn.softplus(dt_raw.astype(jnp.float32) + dt_bias.astype(jnp.float32))
    a_neg = -jnp.exp(a_log.astype(jnp.float32))
    y = ssd_chunked_scan(xs, dt, a_neg, bm, cm)
    y = y + xs * d_skip.astype(jnp.float32)[:, None]
    yg = (y.reshape(bsz, s, SSD_INNER) * jax.nn.silu(z.astype(jnp.float32)))
    yg = yg.reshape(bsz, s, SSD_GROUPS, SSD_INNER // SSD_GROUPS)
    yg = yg * lax.rsqrt(jnp.mean(yg * yg, axis=-1, keepdims=True) + RMS_EPS)
    y_out = yg.reshape(bsz, s, SSD_INNER).astype(h.dtype) * norm_g
    return y_out @ w_o


def swiglu(h, w_gate, w_up, w_down):
    return (jax.nn.silu(h @ w_gate) * (h @ w_up)) @ w_down


def moe_swiglu(h, router, w_gate, w_up, w_down):
    bsz, s, d = h.shape
    t = bsz * s
    xf = h.reshape(t, d)
    probs = jax.nn.softmax((xf @ router).astype(jnp.float32), axis=-1)
    top_p, top_i = lax.top_k(probs, TOP_K)
    gates = top_p / jnp.sum(top_p, axis=-1, keepdims=True)
    n_assign = t * TOP_K
    e_flat = top_i.reshape(-1).astype(jnp.int32)
    tok_flat = jnp.arange(n_assign, dtype=jnp.int32) // TOP_K
    g_flat = gates.reshape(-1)
    order = jnp.argsort(e_flat * n_assign + jnp.arange(n_assign, dtype=jnp.int32))
    e_sorted = e_flat[order]
    counts = jnp.bincount(e_flat, length=N_EXPERTS)
    starts = jnp.cumsum(counts) - counts
    padded = ((counts + MOE_BLOCK - 1) // MOE_BLOCK) * MOE_BLOCK
    pad_ends = jnp.cumsum(padded)
    pad_starts = pad_ends - padded
    dest = pad_starts[e_sorted] + (jnp.arange(n_assign) - starts[e_sorted])
    n_blocks = -(-n_assign // MOE_BLOCK) + N_EXPERTS
    cap = n_blocks * MOE_BLOCK
    slot_tok = jnp.full((cap,), t, jnp.int32).at[dest].set(tok_flat[order])
    slot_gate = jnp.zeros((cap,), jnp.float32).at[dest].set(g_flat[order])
    block_expert = jnp.minimum(jnp.searchsorted(pad_ends, jnp.arange(n_blocks) * MOE_BLOCK, side='right'),
                               N_EXPERTS - 1)
    x_pad = jnp.concatenate([xf, jnp.zeros((1, d), xf.dtype)], axis=0)
    xb = x_pad[slot_tok].reshape(n_blocks, MOE_BLOCK, d)

    def expert_block(args):
        xblk, e = args
        return swiglu(xblk, w_gate[e], w_up[e], w_down[e])

    yb = lax.map(expert_block, (xb, block_expert)).reshape(cap, d)
    y = jax.ops.segment_sum(yb * slot_gate[:, None].astype(yb.dtype), slot_tok, num_segments=t + 1)[:t]
    return y.reshape(bsz, s, d)


def setup_inputs(seed: int = 0) -> dict:
    key = jax.random.key(seed)
    ks = jax.random.split(key, 26)
    nrm = lambda k, shape, sc: jax.random.normal(k, shape, jnp.float32) * sc
    dt0 = jnp.exp(jax.random.uniform(ks[11], (N_B, SSD_HEADS), jnp.float32, math.log(1e-3), math.log(1e-1)))
    return {
        "x": nrm(ks[0], (BATCH, SEQ, D_MODEL), 1.0),
        "positions": jnp.arange(SEQ, dtype=jnp.int32)[None, :] + jax.random.randint(ks[1], (BATCH, 1), 0, 1024, dtype=jnp.int32),
        "mla_w_in": nrm(ks[2], (N_A, D_MODEL, MLA_IN_DIM), D_MODEL ** -0.5),
        "mla_q_norm": 1.0 + nrm(ks[3], (N_A, MLA_Q_RANK), 0.02),
        "mla_kv_norm": 1.0 + nrm(ks[4], (N_A, MLA_KV_RANK), 0.02),
        "mla_w_uq": nrm(ks[5], (N_A, MLA_Q_RANK, MLA_HEADS * (MLA_NOPE + MLA_ROPE)), MLA_Q_RANK ** -0.5),
        "mla_w_ukv": nrm(ks[6], (N_A, MLA_KV_RANK, MLA_HEADS * (MLA_NOPE + MLA_V)), MLA_KV_RANK ** -0.5),
        "mla_w_o": nrm(ks[7], (N_A, MLA_HEADS * MLA_V, D_MODEL), (MLA_HEADS * MLA_V) ** -0.5),
        "ssd_w_in": nrm(ks[8], (N_B, D_MODEL, SSD_IN_DIM), D_MODEL ** -0.5),
        "ssd_conv_w": nrm(ks[9], (N_B, SSD_CONV, SSD_CONV_DIM), SSD_CONV ** -0.5),
        "ssd_conv_b": nrm(ks[10], (N_B, SSD_CONV_DIM), 0.02),
        "ssd_dt_bias": dt0 + jnp.log(-jnp.expm1(-dt0)),
        "ssd_a_log": jnp.log(jax.random.uniform(ks[12], (N_B, SSD_HEADS), jnp.float32, 1.0, 16.0)),
        "ssd_d": 1.0 + nrm(ks[13], (N_B, SSD_HEADS), 0.1),
        "ssd_norm": 1.0 + nrm(ks[14], (N_B, SSD_INNER), 0.02),
        "ssd_w_o": nrm(ks[15], (N_B, SSD_INNER, D_MODEL), SSD_INNER ** -0.5),
        "ffn_w_gate": nrm(ks[16], (N_A, D_MODEL, FFN_DIM), D_MODEL ** -0.5),
        "ffn_w_up": nrm(ks[17], (N_A, D_MODEL, FFN_DIM), D_MODEL ** -0.5),
        "ffn_w_down": nrm(ks[18], (N_A, FFN_DIM, D_MODEL), FFN_DIM ** -0.5),
        "moe_router": nrm(ks[19], (N_B, D_MODEL, N_EXPERTS), D_MODEL ** -0.5),
        "moe_w_gate": nrm(ks[20], (N_B, N_EXPERTS, D_MODEL, FFN_DIM), D_MODEL ** -0.5),
        "moe_w_up": nrm(ks[21], (N_B, N_EXPERTS, D_MODEL, FFN_DIM), D_MODEL ** -0.5),
        "moe_w_down": nrm(ks[22], (N_B, N_EXPERTS, FFN_DIM, D_MODEL), FFN_DIM ** -0.5),
        "norm_mix": 1.0 + nrm(ks[23], (DEPTH, D_MODEL), 0.02),
        "norm_ffn": 1.0 + nrm(ks[24], (DEPTH, D_MODEL), 0.02),
        "norm_final": 1.0 + nrm(ks[25], (D_MODEL,), 0.02),
    }


def reference(x, positions, mla_w_in, mla_q_norm, mla_kv_norm, mla_w_uq, mla_w_ukv, mla_w_o,
              ssd_w_in, ssd_conv_w, ssd_conv_b, ssd_dt_bias, ssd_a_log, ssd_d, ssd_norm, ssd_w_o,
              ffn_w_gate, ffn_w_up, ffn_w_down, moe_router, moe_w_gate, moe_w_up, moe_w_down,
              norm_mix, norm_ffn, norm_final):
    for i in range(DEPTH):
        j = i // 2
        hn = rmsnorm(x, norm_mix[i])
        if i % N_MIXERS == 0:
            mix = mla_mixer(hn, positions, mla_w_in[j], mla_q_norm[j], mla_kv_norm[j],
                            mla_w_uq[j], mla_w_ukv[j], mla_w_o[j])
        else:
            mix = ssd_mixer(hn, ssd_w_in[j], ssd_conv_w[j], ssd_conv_b[j], ssd_dt_bias[j],
                            ssd_a_log[j], ssd_d[j], ssd_norm[j], ssd_w_o[j])
        x = x + mix
        hn = rmsnorm(x, norm_ffn[i])
        if i % 2 == 0:
            ffn = swiglu(hn, ffn_w_gate[j], ffn_w_up[j], ffn_w_down[j])
        else:
            ffn = moe_swiglu(hn, moe_router[j], moe_w_gate[j], moe_w_up[j], moe_w_down[j])
        x = x + ffn
    return rmsnorm(x, norm_final)
```

```python
from contextlib import ExitStack
import numpy as np
import concourse.bass as bass
import concourse.mybir as mybir

F32 = mybir.dt.float32
BF16 = mybir.dt.bfloat16
I32 = mybir.dt.int32
AF = mybir.ActivationFunctionType
ALU = mybir.AluOpType
AX = mybir.AxisListType

ENGS = ("pe", "act", "dve", "pool", "sp")


class Prog:
    def __init__(self):
        self.nc = bass.Bass("TRN2", target_bir_lowering=False)
        self.ops = []
        self.last_w = {}
        self.readers = {}
        self.stack = ExitStack()
        self.n_sb = 0
        self.frontier = set()

    def dram(self, name, shape, dtype, kind="Internal"):
        return self.nc.dram_tensor(name, list(shape), dtype, kind=kind).ap()

    def sb(self, name, shape, dtype):
        t = self.stack.enter_context(self.nc.sbuf_tensor("s_" + name, list(shape), dtype))
        return t

    def ps(self, name, shape, dtype):
        t = self.stack.enter_context(self.nc.psum_tensor("p_" + name, list(shape), dtype))
        return t

    def op(self, eng, fn, r=(), w=(), dma=None):
        i = len(self.ops)
        deps = set()
        for k in r:
            if k in self.last_w:
                deps.add(self.last_w[k])
        for k in w:
            if k in self.last_w:
                deps.add(self.last_w[k])
            for x in self.readers.get(k, ()):
                deps.add(x)
        deps |= self.frontier
        deps.discard(i)
        self.ops.append(dict(eng=eng, fn=fn, deps=deps, dma=dma, inc=(dma is not None)))
        for k in r:
            self.readers.setdefault(k, []).append(i)
        for k in w:
            self.last_w[k] = i
            self.readers[k] = []
        return i

    def barrier(self):
        fr = {}
        for i, o in enumerate(self.ops):
            k = ("dma", o["dma"]) if o["dma"] is not None else ("eng", o["eng"])
            fr[k] = i
        self.frontier = set(fr.values())

    def dma(self, q, out, in_, r=(), w=(), sem=None, **kw):
        assert sem is not None
        return self.op(q, lambda e: e.dma_start(out=out, in_=in_, **kw), r=r, w=w, dma=sem)

    def emit(self, final_wait_ops=()):
        nc = self.nc
        ops = self.ops
        for o in ops:
            nd = set()
            for d in o["deps"]:
                if ops[d]["dma"] is None and ops[d]["eng"] == "pe" and o["eng"] == "pe" and o["dma"] is None:
                    continue
                nd.add(d)
            o["deps"] = nd
            for d in nd:
                ops[d]["inc"] = True
        semnames = []
        for e in ("pe", "act", "dve", "pool"):
            semnames.append("c_" + e)
        for o in ops:
            if o["dma"] is not None and ("d_" + str(o["dma"])) not in semnames:
                semnames.append("d_" + str(o["dma"]))
        sems = {n: self.stack.enter_context(nc.semaphore(n)) for n in semnames}
        counts = {n: 0 for n in semnames}
        for o in ops:
            if o["dma"] is not None:
                n = "d_" + str(o["dma"])
                counts[n] += 16
                o["tok"] = (n, counts[n])
                o["grp"] = str(o["dma"]).startswith("G:")
            elif o["inc"]:
                n = "c_" + o["eng"]
                counts[n] += 1
                o["tok"] = (n, counts[n])
        self.counts = counts
        for o in ops:
            if o.get("grp"):
                o["tok"] = (o["tok"][0], counts[o["tok"][0]])
        per_eng = {e: [] for e in ENGS}
        for i, o in enumerate(ops):
            per_eng[o["eng"]].append(i)
        final_tokens = [ops[i]["tok"] for i in final_wait_ops]
        final_tokens += [(n, v) for n, v in counts.items() if n.startswith("d_") and v > 0]

        def run_engine(ename, eobj, extra_final=False):
            waited = {}
            for i in per_eng[ename]:
                o = ops[i]
                need = {}
                for d in o["deps"]:
                    n, v = ops[d]["tok"]
                    if need.get(n, 0) < v:
                        need[n] = v
                for n, v in need.items():
                    if waited.get(n, 0) < v:
                        eobj.wait_ge(sems[n], v)
                        waited[n] = v
                ins = o["fn"](eobj)
                if o["inc"]:
                    n, v = o["tok"]
                    ins.then_inc(sems[n], 16 if o["dma"] is not None else 1)
            if extra_final:
                need = {}
                for n, v in final_tokens:
                    if need.get(n, 0) < v:
                        need[n] = v
                for n, v in need.items():
                    if waited.get(n, 0) < v:
                        eobj.wait_ge(sems[n], v)

        with nc.Block() as block:
            @block.tensor
            def _(e):
                run_engine("pe", e)

            @block.scalar
            def _(e):
                run_engine("act", e)

            @block.vector
            def _(e):
                run_engine("dve", e)

            @block.gpsimd
            def _(e):
                run_engine("pool", e)

            @block.sync
            def _(e):
                run_engine("sp", e, extra_final=True)
        self.stack.close()
        return nc


D = 2048
EPS = 1e-6


class Ctx:
    def __init__(self, nring=6, ring_elems=8192):
        P = self.P = Prog()
        self.nc = P.nc
        self.cin = {}
        self.identf_d = P.dram("c_identf", [128, 128], F32, kind="ExternalInput")
        self.identb_d = P.dram("c_identb", [128, 128], BF16, kind="ExternalInput")
        self.identf = P.sb("identf", [128, 128], F32)
        self.identb = P.sb("identb", [128, 128], BF16)
        P.dma("sp", self.identf[:], self.identf_d, w=["identf"], sem="G:consts")
        P.dma("sp", self.identb[:], self.identb_d, w=["identb"], sem="G:consts")
        self.epsb = P.sb("epsb", [128, 1], F32)
        P.op("dve", lambda e: e.memset(self.epsb[:], EPS), w=["epsb"])
        self.ps = [P.ps(f"ps{i}", [128, 512], F32) for i in range(8)]
        self.ring = [P.sb(f"ring{i}", [128, ring_elems], BF16) for i in range(nring)]
        self.nring = nring
        self.ring_i = 0
        self.small_i = 0

    def consts_np(self):
        import ml_dtypes
        d = {"c_identf": np.eye(128, dtype=np.float32),
             "c_identb": np.eye(128, dtype=np.float32).astype(ml_dtypes.bfloat16)}
        d.update(self.cin)
        return d

    def wload(self, src_ap, shape_str=None, **kw):
        i = self.ring_i % self.nring
        self.ring_i += 1
        key = f"ring{i}"
        shp = src_ap.shape
        n = 1
        for s in shp[1:]:
            n *= s
        view = self.ring[i][:, 0:n]
        if len(shp) == 3:
            view = view.rearrange("p (a b) -> p a b", a=shp[1])
        self.P.dma("pool", view, src_ap, w=[key], sem=key)
        return key, view

    def bcast_load(self, name, vec_ap, n):
        t = self.P.sb(name, [128, n], F32)
        self.P.dma("sp", t[:], vec_ap.partition_broadcast(128), w=[name], sem="G:consts")
        return t

    def rstd(self, x_ap, xkey, n, junk, junk_key, out_t, out_key):
        P = self.P
        P.op("act", lambda e: e.activation(out=junk, in_=x_ap, func=AF.Square, scale=float(n) ** -0.5,
                                           accum_out=out_t[:]), r=[xkey], w=[junk_key, out_key])
        P.op("act", lambda e: e.activation(out=out_t[:], in_=out_t[:], func=AF.Sqrt, bias=self.epsb[:]),
             r=[out_key, "epsb"], w=[out_key])
        P.op("dve", lambda e: e.reciprocal(out=out_t[:], in_=out_t[:]), r=[out_key], w=[out_key])

    def transpose_to(self, src_t, src_key, nchunks, dst_fn, dst_key, banks, f32=True, part=128):
        P = self.P
        ident = self.identf if f32 else self.identb
        ikey = "identf" if f32 else "identb"
        per = 4
        gi = 0
        for c0 in range(0, nchunks, per):
            n = min(per, nchunks - c0)
            bk = banks[gi % len(banks)]
            gi += 1
            pst = self.ps[bk]
            pv = pst[:] if f32 else pst[:].bitcast(BF16)
            for j in range(n):
                c = c0 + j
                P.op("pe", lambda e, c=c, j=j, pv=pv: e.transpose(
                    out=pv[:, j * part:(j + 1) * part], in_=src_t[0:part, c * 128:(c + 1) * 128],
                    identity=ident[0:part, 0:part]), r=[src_key, ikey], w=[f"ps{bk}"])
            src = pv[:, 0:n * part].rearrange("p (c t) -> p c t", c=n)
            dst = dst_fn(c0, n)
            if gi % 2 == 0:
                P.op("act", lambda e, src=src, dst=dst: e.activation(out=dst, in_=src, func=AF.Copy),
                     r=[f"ps{bk}"], w=[dst_key])
            else:
                P.op("dve", lambda e, src=src, dst=dst: e.tensor_copy(out=dst, in_=src),
                     r=[f"ps{bk}"], w=[dst_key])


FF = 7168


def ffn_core(C, hT, hkey, T, tts, wg_d, wu_d, wd_d, acc_fn, psG, psU, psD, a_bufs, sg_bufs, HB=512, akeys=("a0", "a1"), sgkeys=("sg0", "sg1")):
    P = C.P
    nsplit = [(n0, min(512, T - n0)) for n0 in range(0, T, 512)]
    if T > 512 and T % 2 == 0 and T // 2 <= 512:
        nsplit = [(0, T // 2), (T // 2, T // 2)]
    gi = 0
    di = 0
    NJ = HB // 128
    for hb in range(FF // HB):
        kg, vg = C.wload(wg_d[:, hb * HB:(hb + 1) * HB].rearrange("(c p) n -> p c n", p=128))
        ku, vu = C.wload(wu_d[:, hb * HB:(hb + 1) * HB].rearrange("(c p) n -> p c n", p=128))
        kd, vd = C.wload(wd_d[hb * HB:(hb + 1) * HB, :].rearrange("(c p) n -> p c n", p=128))
        ab = hb % 2
        a_t = a_bufs[ab]
        akey = akeys[ab]
        for j in range(NJ):
            for (n0, nn) in nsplit:
                bg = psG[gi % len(psG)]
                bu = psU[gi % len(psU)]
                sgb = gi % 2
                gi += 1
                for kc in range(16):
                    P.op("pe", lambda e, kc=kc, j=j, n0=n0, nn=nn, bg=bg, vg=vg: e.matmul(
                        out=C.ps[bg][:, 0:nn], lhsT=vg[:, kc, j * 128:(j + 1) * 128], rhs=hT[:, kc, n0:n0 + nn],
                        start=(kc == 0), stop=(kc == 15)), r=[kg, hkey], w=[f"ps{bg}"])
                for kc in range(16):
                    P.op("pe", lambda e, kc=kc, j=j, n0=n0, nn=nn, bu=bu, vu=vu: e.matmul(
                        out=C.ps[bu][:, 0:nn], lhsT=vu[:, kc, j * 128:(j + 1) * 128], rhs=hT[:, kc, n0:n0 + nn],
                        start=(kc == 0), stop=(kc == 15)), r=[ku, hkey], w=[f"ps{bu}"])
                P.op("act", lambda e, bg=bg, nn=nn, sgb=sgb: e.activation(
                    out=sg_bufs[sgb][:, 0:nn], in_=C.ps[bg][:, 0:nn], func=AF.Silu), r=[f"ps{bg}"], w=[sgkeys[sgb]])
                P.op("dve", lambda e, bu=bu, nn=nn, n0=n0, j=j, sgb=sgb, a_t=a_t: e.tensor_tensor(
                    out=a_t[:, j, n0:n0 + nn], in0=C.ps[bu][:, 0:nn], in1=sg_bufs[sgb][:, 0:nn], op=ALU.mult),
                     r=[f"ps{bu}", sgkeys[sgb]], w=[akey])
        for ti, (t0, tn) in enumerate(tts):
            for fbk in range(4):
                bd = psD[di % len(psD)]
                di += 1
                for c in range(NJ):
                    P.op("pe", lambda e, c=c, t0=t0, tn=tn, fbk=fbk, bd=bd, a_t=a_t, vd=vd: e.matmul(
                        out=C.ps[bd][0:tn, :], lhsT=a_t[:, c, t0:t0 + tn], rhs=vd[:, c, fbk * 512:(fbk + 1) * 512],
                        start=(c == 0), stop=(c == NJ - 1)), r=[akey, kd], w=[f"ps{bd}"])
                acc, acc_key = acc_fn(ti, fbk)
                P.op("dve", lambda e, bd=bd, tn=tn, acc=acc: e.tensor_tensor(
                    out=acc, in0=C.ps[bd][0:tn, :], in1=acc, op=ALU.add), r=[f"ps{bd}", acc_key], w=[acc_key])


def norm_to_hT(C, x_ap, xkey, gb, gkey, hn_t, hT, hkey, col0, banks, rs_t, rs_key, hnkey="hn"):
    P = C.P
    C.rstd(x_ap, xkey, D, hn_t[:], hnkey, rs_t, rs_key)
    P.op("dve", lambda e: e.scalar_tensor_tensor(out=hn_t[:], in0=x_ap, scalar=rs_t[:], in1=gb[:],
                                                 op0=ALU.mult, op1=ALU.mult),
         r=[xkey, rs_key, gkey], w=[hnkey])
    C.transpose_to(hn_t, hnkey, 16, lambda c0, n: hT[:, c0:c0 + n, col0:col0 + 128], hkey, banks)


def build_l2(T=2048, TB=512, with_wo=True, KO=2048):
    C = Ctx(nring=6)
    P = C.P
    x = P.dram("x", [T, D], F32, kind="ExternalInput")
    oT = P.dram("oT", [KO, T], BF16, kind="ExternalInput")
    w_o = P.dram("w_o", [KO, D], F32, kind="ExternalInput")
    g = P.dram("g", [1, D], F32, kind="ExternalInput")
    wg = P.dram("wg", [D, FF], F32, kind="ExternalInput")
    wu = P.dram("wu", [D, FF], F32, kind="ExternalInput")
    wd = P.dram("wd", [FF, D], F32, kind="ExternalInput")
    x2 = P.dram("x2", [T, D], F32, kind="ExternalOutput")
    KC = KO // 128
    gb = C.bcast_load("gb", g, D)
    xb = P.sb("xb", [128, TB // 128, D], F32)
    hT = P.sb("hT", [128, 16, TB], BF16)
    oTb = P.sb("oTb", [128, KC, TB], BF16)
    a_bufs = [P.sb(f"a{i}", [128, 4, TB], BF16) for i in range(2)]
    sg_bufs = [P.sb(f"sg{i}", [128, 512], F32) for i in range(2)]
    hn_t = P.sb("hn", [128, D], F32)
    rs_t = P.sb("rs", [128, 1], F32)
    ntt = TB // 128
    outs = []
    for tb in range(T // TB):
        t0 = tb * TB
        for tt in range(ntt):
            P.dma("sp", xb[:, tt, :], x[t0 + tt * 128:t0 + (tt + 1) * 128, :], w=[f"xb{tt}"], sem=f"xb{tt}")
        P.dma("sp", oTb[:], oT[:, t0:t0 + TB].rearrange("(c p) t -> p c t", p=128), w=["oTb"], sem="oTb")
        wcb = 8192 // KC
        di = 0
        for cb in range(D // wcb):
            kw, vw = C.wload(w_o[:, cb * wcb:(cb + 1) * wcb].rearrange("(c p) n -> p c n", p=128))
            for tt in range(ntt):
                bd = di % 2
                di += 1
                for kc in range(KC):
                    P.op("pe", lambda e, kc=kc, tt=tt, bd=bd, vw=vw: e.matmul(
                        out=C.ps[bd][:, 0:wcb], lhsT=oTb[:, kc, tt * 128:(tt + 1) * 128], rhs=vw[:, kc, :],
                        start=(kc == 0), stop=(kc == KC - 1)), r=["oTb", kw], w=[f"ps{bd}"])
                P.op("dve", lambda e, tt=tt, cb=cb, bd=bd: e.tensor_tensor(
                    out=xb[:, tt, cb * wcb:(cb + 1) * wcb], in0=C.ps[bd][:, 0:wcb],
                    in1=xb[:, tt, cb * wcb:(cb + 1) * wcb], op=ALU.add), r=[f"ps{bd}", f"xb{tt}"], w=[f"xb{tt}"])
        for tt in range(ntt):
            norm_to_hT(C, xb[:, tt, :], f"xb{tt}", gb, "gb", hn_t, hT, "hT", tt * 128, [2, 3], rs_t, "rs")
        ffn_core(C, hT, "hT", TB, [(tt * 128, 128) for tt in range(ntt)], wg, wu, wd,
                 lambda ti, fbk: (xb[:, ti, fbk * 512:(fbk + 1) * 512], f"xb{ti}"),
                 psG=[4, 5], psU=[6, 7], psD=[0, 1, 2, 3], a_bufs=a_bufs, sg_bufs=sg_bufs)
        for tt in range(ntt):
            outs.append(P.dma("sp", x2[t0 + tt * 128:t0 + (tt + 1) * 128, :], xb[:, tt, :], r=[f"xb{tt}"], sem=f"xout{tt}"))
    nc = P.emit(final_wait_ops=outs)
    return C, nc


import math


S = 4096
NH = 8
SCALE = 192 ** -0.5
TWO_PI = 2 * math.pi
SINSC = TWO_PI * 0.999999


DBG = {}


def build_l1(S=S, NH=NH, phase2=True):
    C = Ctx(nring=0)
    P = C.P
    NT = S // 128
    x = P.dram("x", [S, D], F32, kind="ExternalInput")
    pos_d = P.dram("pos", [128, S // 128], I32, kind="ExternalInput")
    g_d = P.dram("g", [1, D], F32, kind="ExternalInput")
    win_d = P.dram("w_in", [D, 1152], F32, kind="ExternalInput")
    qg_d = P.dram("qg", [1, 512], F32, kind="ExternalInput")
    kvg_d = P.dram("kvg", [1, 512], F32, kind="ExternalInput")
    wuq_d = P.dram("w_uq", [512, NH, 256], F32, kind="ExternalInput")
    wukv_d = P.dram("w_ukv", [512, NH, 256], F32, kind="ExternalInput")
    invf_d = P.dram("invf", [1, 64], F32, kind="ExternalInput")
    tri_d = P.dram("tri", [128, 128], BF16, kind="ExternalInput")
    oT_d = P.dram("oT", [NH * 128, S], BF16, kind="ExternalOutput")

    gb = C.bcast_load("gb", g_d, D)
    qg = C.bcast_load("qgb", qg_d, 512)
    kvg = C.bcast_load("kvgb", kvg_d, 512)
    invf = C.bcast_load("invfb", invf_d, 64)
    tri = P.sb("tri", [128, 128], BF16)
    P.dma("sp", tri[:], tri_d, w=["tri"], sem="G:consts")
    posi = P.sb("posi", [128, NT], I32)
    posf = P.sb("posf", [128, NT], F32)
    P.dma("sp", posi[:], pos_d, w=["posi"], sem="G:consts")
    P.op("dve", lambda e: e.tensor_copy(out=posf[:], in_=posi[:]), r=["posi"], w=["posf"])

    cqT = P.sb("cqT", [128, 4, S], BF16)
    ckvT = P.sb("ckvT", [128, 4, S], BF16)
    kTr = P.sb("kTr", [64, S], BF16)
    cosT = P.sb("cosT", [64, S], F32)
    sinT = P.sb("sinT", [64, S], F32)
    big = P.sb("big", [128, 16 * 1152], BF16)
    win = big[:].rearrange("p (c n) -> p c n", c=16)
    qTn = big[:, 0:S]
    qTr = big[0:64, S:2 * S]
    kTn = big[:, 2 * S:3 * S]
    V = big[:, 3 * S:3 * S + NT * 132].rearrange("p (t n) -> p t n", t=NT)

    xt = [P.sb(f"xt{i}", [128, D], F32) for i in range(2)]
    hn_t = P.sb("hn", [128, D], F32)
    rs_t = P.sb("rs", [128, 1], F32)
    rq_t = P.sb("rq", [128, 1], F32)
    rkv_t = P.sb("rkv", [128, 1], F32)
    hTt = P.sb("hTt", [128, 16, 128], BF16)
    cnq = P.sb("cnq", [128, 512], F32)
    cnkv = P.sb("cnkv", [128, 512], F32)
    junk = P.sb("junk", [128, 512], F32)
    names = ["tt_", "kf", "f", "u", "gq", "cs", "sn", "t1", "t2"]
    rt = {n: P.sb("r_" + n, [128, 64], F32) for n in names}
    ki = P.sb("r_ki", [128, 64], I32)
    kro = P.sb("kro", [128, 64], BF16)

    for q in range(4):
        P.dma("pool", win[:, q * 4:(q + 1) * 4, :], win_d[q * 512:(q + 1) * 512, :].rearrange("(c p) n -> p c n", p=128),
              w=[f"win{q}"], sem=f"win{q}")

    for tt in range(NT):
        b = tt % 2
        xk = f"xt{b}"
        c0 = tt * 128
        P.dma("sp", xt[b][:], x[c0:c0 + 128, :], w=[xk], sem=xk)
        if DBG.get("nonorm"):
            continue
        C.rstd(xt[b][:], xk, D, hn_t[:], "hn", rs_t, "rs")
        P.op("dve", lambda e, b=b: e.scalar_tensor_tensor(out=hn_t[:], in0=xt[b][:], scalar=rs_t[:], in1=gb[:],
                                                          op0=ALU.mult, op1=ALU.mult), r=[xk, "rs", "gb"], w=["hn"])
        C.transpose_to(hn_t, "hn", 16, lambda k0, n: hTt[:, k0:k0 + n, :], "hTt", [0, 1])
        for (bk, n0, nn) in (() if DBG.get("nomm") else ((2, 0, 512), (3, 512, 512), (4, 1024, 128))):
            for kc in range(16):
                P.op("pe", lambda e, kc=kc, bk=bk, n0=n0, nn=nn: e.matmul(
                    out=C.ps[bk][:, 0:nn], lhsT=hTt[:, kc, :], rhs=win[:, kc, n0:n0 + nn],
                    start=(kc == 0), stop=(kc == 15)), r=["hTt", f"win{kc // 4}"], w=[f"ps{bk}"])
        for (bk, rst, rkey, gt, gkey, cn, cnkey, dstT, dkey, tb) in (() if DBG.get("nolat") else (
                (2, rq_t, "rq", qg, "qgb", cnq, "cnq", cqT, "cqT", 5),
                (3, rkv_t, "rkv", kvg, "kvgb", cnkv, "cnkv", ckvT, "ckvT", 6))):
            if DBG.get("lat_nostat"):
                P.op("dve", lambda e, bk=bk, cn=cn: e.tensor_copy(out=cn[:], in_=C.ps[bk][:]), r=[f"ps{bk}"], w=[cnkey])
            else:
                C.rstd(C.ps[bk][:], f"ps{bk}", 512, junk[:], "junk", rst, rkey)
                P.op("dve", lambda e, bk=bk, rst=rst, gt=gt, cn=cn: e.scalar_tensor_tensor(
                    out=cn[:], in0=C.ps[bk][:], scalar=rst[:], in1=gt[:], op0=ALU.mult, op1=ALU.mult),
                     r=[f"ps{bk}", rkey, gkey], w=[cnkey])
            C.transpose_to(cn, cnkey, 4, lambda k0, n, dstT=dstT, c0=c0: dstT[:, k0:k0 + n, c0:c0 + 128], dkey, [tb])
        if DBG.get("norope"):
            continue
        pass
        pcol = posf[:, tt:tt + 1]
        R = rt
        P.op("dve", lambda e, pcol=pcol: e.tensor_scalar(out=R["tt_"][:], in0=invf[:], scalar1=pcol, scalar2=1.0 / TWO_PI,
                                                         op0=ALU.mult, op1=ALU.mult), r=["posf", "invfb"], w=["r_tt_"])
        P.op("dve", lambda e: e.tensor_copy(out=ki[:], in_=R["tt_"][:]), r=["r_tt_"], w=["r_ki"])
        P.op("dve", lambda e: e.tensor_copy(out=R["kf"][:], in_=ki[:]), r=["r_ki"], w=["r_kf"])
        P.op("dve", lambda e: e.tensor_tensor(out=R["f"][:], in0=R["tt_"][:], in1=R["kf"][:], op=ALU.subtract),
             r=["r_tt_", "r_kf"], w=["r_f"])
        P.op("dve", lambda e: e.tensor_scalar(out=R["u"][:], in0=R["f"][:], scalar1=0.5, scalar2=None, op0=ALU.is_gt),
             r=["r_f"], w=["r_u"])
        P.op("dve", lambda e: e.tensor_tensor(out=R["f"][:], in0=R["f"][:], in1=R["u"][:], op=ALU.subtract),
             r=["r_f", "r_u"], w=["r_f"])
        P.op("dve", lambda e: e.tensor_scalar(out=R["u"][:], in0=R["f"][:], scalar1=-0.5, scalar2=None, op0=ALU.is_lt),
             r=["r_f"], w=["r_u"])
        P.op("dve", lambda e: e.tensor_tensor(out=R["f"][:], in0=R["f"][:], in1=R["u"][:], op=ALU.add),
             r=["r_f", "r_u"], w=["r_f"])
        P.op("dve", lambda e: e.tensor_scalar(out=R["gq"][:], in0=R["f"][:], scalar1=0.25, scalar2=None, op0=ALU.add),
             r=["r_f"], w=["r_gq"])
        P.op("dve", lambda e: e.tensor_scalar(out=R["u"][:], in0=R["gq"][:], scalar1=0.5, scalar2=None, op0=ALU.is_gt),
             r=["r_gq", "r_f"], w=["r_u"])
        P.op("dve", lambda e: e.tensor_tensor(out=R["gq"][:], in0=R["gq"][:], in1=R["u"][:], op=ALU.subtract),
             r=["r_gq", "r_u"], w=["r_gq"])
        P.op("act", lambda e: e.activation(out=R["sn"][:, 0:32], in_=R["f"][:, 0:32], func=AF.Sin, scale=-SINSC),
             r=["r_f"], w=["r_sn"])
        P.op("act", lambda e: e.activation(out=R["sn"][:, 32:64], in_=R["f"][:, 32:64], func=AF.Sin, scale=SINSC),
             r=["r_f"], w=["r_sn"])
        P.op("act", lambda e: e.activation(out=R["cs"][:], in_=R["gq"][:], func=AF.Sin, scale=SINSC),
             r=["r_gq"], w=["r_cs"])
        P.op("dve", lambda e: e.tensor_tensor(out=R["t1"][:], in0=C.ps[4][:, 0:64], in1=R["cs"][:], op=ALU.mult),
             r=["ps4", "r_cs"], w=["r_t1"])
        P.op("dve", lambda e: e.tensor_tensor(out=R["t2"][:], in0=C.ps[4][:, 64:128], in1=R["sn"][:], op=ALU.mult),
             r=["ps4", "r_sn"], w=["r_t2"])
        P.op("dve", lambda e: e.tensor_tensor(out=kro[:], in0=R["t1"][:], in1=R["t2"][:], op=ALU.add),
             r=["r_t1", "r_t2"], w=["kro"])
        pvb = C.ps[7][:].bitcast(BF16)
        P.op("pe", lambda e, pvb=pvb: e.transpose(out=pvb[0:64, 0:128], in_=kro[:, 0:64], identity=C.identb[:]),
             r=["kro", "identb"], w=["ps7"])
        P.op("act", lambda e, pvb=pvb, c0=c0: e.activation(out=kTr[:, c0:c0 + 128], in_=pvb[0:64, 0:128], func=AF.Copy),
             r=["ps7"], w=["kTr"])
        for (src, skey, dst, dkey) in ((R["cs"], "r_cs", cosT, "cosT"), (R["sn"], "r_sn", sinT, "sinT")):
            P.op("pe", lambda e, src=src: e.transpose(out=C.ps[7][0:64, 0:128], in_=src[:, 0:64], identity=C.identf[:]),
                 r=[skey, "identf"], w=["ps7"])
            P.op("dve", lambda e, dst=dst, c0=c0: e.tensor_copy(out=dst[:, c0:c0 + 128], in_=C.ps[7][0:64, 0:128]),
                 r=["ps7"], w=[dkey])

    P.barrier()
    if not phase2:
        o1 = P.dma("sp", oT_d[0:128, :], cqT[:, 0, :], r=["cqT"], sem="dbg1")
        o2 = P.dma("sp", oT_d[128:192, :], kTr[:], r=["kTr"], sem="dbg2")
        return C, P.emit(final_wait_ops=[o1, o2])
    wq = [P.sb(f"wq{i}", [128, 4, 256], BF16) for i in range(2)]
    wkv = [P.sb(f"wkv{i}", [128, 4, 256], BF16) for i in range(2)]
    pt = [P.sb(f"pt{i}", [128, 512], BF16) for i in range(3)]
    rt1 = P.sb("rt1", [64, 512], F32)
    rt2 = P.sb("rt2", [64, 512], F32)
    rec = [P.sb(f"rec{i}", [128, 1], F32) for i in range(2)]
    osb = [P.sb(f"osb{i}", [128, 128], BF16) for i in range(2)]
    oTs = [P.sb(f"oTs{i}", [128, 512], BF16) for i in range(2)]
    P.op("dve", lambda e: e.memset(V[:, :, 128:132], 1.0), w=["V"])
    outs = []
    pti = 0
    sci = 0
    cpi = 0

    def evac(src, dst, rkeys, wkeys):
        nonlocal cpi
        cpi += 1
        if cpi % 2 == 0:
            P.op("act", lambda e: e.activation(out=dst, in_=src, func=AF.Copy), r=rkeys, w=wkeys)
        else:
            P.op("dve", lambda e: e.tensor_copy(out=dst, in_=src), r=rkeys, w=wkeys)

    for h in range(NH):
        hb = h % 2
        P.dma("pool", wq[hb][:], wuq_d[:, h, :].rearrange("(c p) n -> p c n", p=128), w=[f"wq{hb}"], sem=f"wq{hb}")
        P.dma("pool", wkv[hb][:], wukv_d[:, h, :].rearrange("(c p) n -> p c n", p=128), w=[f"wkv{hb}"], sem=f"wkv{hb}")
        pj = 0
        for t8 in range(S // 512):
            cs_ = slice(t8 * 512, (t8 + 1) * 512)
            bk = 6 + (pj % 2); pj += 1
            for kc in range(4):
                P.op("pe", lambda e, kc=kc, bk=bk, cs_=cs_, hb=hb: e.matmul(
                    out=C.ps[bk][:], lhsT=wkv[hb][:, kc, 0:128], rhs=ckvT[:, kc, cs_], start=(kc == 0), stop=(kc == 3)),
                     r=[f"wkv{hb}", "ckvT"], w=[f"ps{bk}"])
            evac(C.ps[bk][:], kTn[:, cs_], [f"ps{bk}"], ["kTn"])
            bk = 6 + (pj % 2); pj += 1
            for kc in range(4):
                P.op("pe", lambda e, kc=kc, bk=bk, cs_=cs_, hb=hb: e.matmul(
                    out=C.ps[bk][:], lhsT=wq[hb][:, kc, 0:128], rhs=cqT[:, kc, cs_], start=(kc == 0), stop=(kc == 3)),
                     r=[f"wq{hb}", "cqT"], w=[f"ps{bk}"])
            evac(C.ps[bk][:], qTn[:, cs_], [f"ps{bk}"], ["qTn"])
            for (bk, c_lo) in ((6, 128), (7, 192)):
                for kc in range(4):
                    P.op("pe", lambda e, kc=kc, bk=bk, cs_=cs_, hb=hb, c_lo=c_lo: e.matmul(
                        out=C.ps[bk][0:64, :], lhsT=wq[hb][:, kc, c_lo:c_lo + 64], rhs=cqT[:, kc, cs_],
                        start=(kc == 0), stop=(kc == 3)), r=[f"wq{hb}", "cqT"], w=[f"ps{bk}"])
            P.op("dve", lambda e, cs_=cs_: e.tensor_tensor(out=rt1[:], in0=C.ps[6][0:64, :], in1=cosT[:, cs_], op=ALU.mult),
                 r=["ps6", "cosT"], w=["rt1"])
            P.op("dve", lambda e, cs_=cs_: e.tensor_tensor(out=rt2[:], in0=C.ps[7][0:64, :], in1=sinT[:, cs_], op=ALU.mult),
                 r=["ps7", "sinT"], w=["rt2"])
            P.op("dve", lambda e, cs_=cs_: e.tensor_tensor(out=qTr[:, cs_], in0=rt1[:], in1=rt2[:], op=ALU.add),
                 r=["rt1", "rt2"], w=["qTr"])
            pj = 0
            bk = 6
            for j in range(4):
                kt = t8 * 4 + j
                for kc in range(4):
                    P.op("pe", lambda e, kc=kc, kt=kt, j=j, hb=hb: e.matmul(
                        out=C.ps[6][:, j * 128:(j + 1) * 128], lhsT=ckvT[:, kc, kt * 128:(kt + 1) * 128],
                        rhs=wkv[hb][:, kc, 128:256], start=(kc == 0), stop=(kc == 3)),
                         r=[f"wkv{hb}", "ckvT"], w=["ps6"])
            evac(C.ps[6][:].rearrange("p (j n) -> p j n", j=4), V[:, t8 * 4:(t8 + 1) * 4, 0:128], ["ps6"], ["V"])
        for qt in range(S // 512):
            q0 = qt * 512
            def o_ps(j):
                return C.ps[2 + j][:, 0:129], f"ps{2 + j}"

            nk = 4 * qt + 4
            for kt in range(nk):
                i = kt - 4 * qt
                jmin = max(i, 0)
                n0 = jmin * 128
                nn = 512 - n0
                sb_ = sci % 2; sci += 1
                P.op("pe", lambda e, kt=kt, sb_=sb_, n0=n0, nn=nn, q0=q0: e.matmul(
                    out=C.ps[sb_][:, 0:nn], lhsT=kTn[:, kt * 128:(kt + 1) * 128], rhs=qTn[:, q0 + n0:q0 + 512],
                    start=True, stop=False), r=["kTn", "qTn"], w=[f"ps{sb_}"])
                P.op("pe", lambda e, kt=kt, sb_=sb_, n0=n0, nn=nn, q0=q0: e.matmul(
                    out=C.ps[sb_][:, 0:nn], lhsT=kTr[:, kt * 128:(kt + 1) * 128], rhs=qTr[:, q0 + n0:q0 + 512],
                    start=False, stop=True), r=["kTr", "qTr"], w=[f"ps{sb_}"])
                pb = pti % 3; pti += 1
                P.op("act", lambda e, sb_=sb_, nn=nn, pb=pb: e.activation(
                    out=pt[pb][:, 0:nn], in_=C.ps[sb_][:, 0:nn], func=AF.Exp, scale=SCALE), r=[f"ps{sb_}"], w=[f"pt{pb}"])
                if i >= 0:
                    P.op("dve", lambda e, pb=pb: e.tensor_tensor(out=pt[pb][:, 0:128], in0=pt[pb][:, 0:128], in1=tri[:],
                                                                 op=ALU.mult), r=[f"pt{pb}", "tri"], w=[f"pt{pb}"])
                for j in range(jmin, 4):
                    oap, okey = o_ps(j)
                    P.op("pe", lambda e, j=j, pb=pb, n0=n0, oap=oap, kt=kt, qt=qt: e.matmul(
                        out=oap, lhsT=pt[pb][:, j * 128 - n0:(j + 1) * 128 - n0], rhs=V[:, kt, 0:129],
                        start=(kt == 0), stop=(kt == 4 * qt + j)), r=[f"pt{pb}", "V"], w=[okey])
            osl = qt % 2
            for j in range(4):
                oap, okey = o_ps(j)
                rb = j % 2
                P.op("dve", lambda e, oap=oap, rb=rb: e.reciprocal(out=rec[rb][:], in_=oap[:, 128:129]),
                     r=[okey], w=[f"rec{rb}"])
                P.op("dve", lambda e, oap=oap, rb=rb: e.tensor_scalar(out=osb[rb][:], in0=oap[:, 0:128], scalar1=rec[rb][:],
                                                                      scalar2=None, op0=ALU.mult),
                     r=[okey, f"rec{rb}"], w=[f"osb{rb}"])
                tb = 6 + (j % 2)
                pvb = C.ps[tb][:].bitcast(BF16)
                P.op("pe", lambda e, rb=rb, pvb=pvb: e.transpose(out=pvb[:, 0:128], in_=osb[rb][:], identity=C.identb[:]),
                     r=[f"osb{rb}", "identb"], w=[f"ps{tb}"])
                evac(pvb[:, 0:128], oTs[osl][:, j * 128:(j + 1) * 128], [f"ps{tb}"], [f"oTs{osl}"])
            outs.append(P.dma("sp", oT_d[h * 128:(h + 1) * 128, q0:q0 + 512], oTs[osl][:], r=[f"oTs{osl}"],
                              sem=f"oTs{osl}"))
    nc = P.emit(final_wait_ops=outs)
    return C, nc


L = 256
NHC = 32
NCH = 16
WCOLS = 5152


def build_l3(S=4096):
    C = Ctx(nring=3)
    P = C.P
    NCK = S // L
    x2 = P.dram("x2", [S, D], F32, kind="ExternalInput")
    g_d = P.dram("g", [1, D], F32, kind="ExternalInput")
    w_d = P.dram("w_cat", [D, WCOLS], F32, kind="ExternalInput")
    cw_d = P.dram("cw", [128, 24, 4], F32, kind="ExternalInput")
    cb_d = P.dram("cb", [128, 24], F32, kind="ExternalInput")
    dtb_d = P.dram("dtb", [1, 32], F32, kind="ExternalInput")
    alog_d = P.dram("alog", [1, 32], F32, kind="ExternalInput")
    dcol_d = P.dram("dcol", [128, 16], F32, kind="ExternalInput")
    ng_d = P.dram("ng", [128, 16], F32, kind="ExternalInput")
    tm_d = P.dram("c_tm", [128, 256], F32, kind="ExternalInput")
    cm_d = P.dram("c_cm", [128, 384], F32, kind="ExternalInput")
    y_d = P.dram("yT", [2048, S], BF16, kind="ExternalOutput")

    def cload(name, src, shape, dt=F32):
        t = P.sb(name, shape, dt)
        P.dma("sp", t[:], src, w=[name], sem="G:consts")
        return t

    gb = C.bcast_load("gb", g_d, D)
    dtb = C.bcast_load("dtbb", dtb_d, 32)
    aneg = C.bcast_load("aneg", alog_d, 32)
    cw = cload("cw", cw_d, [128, 24, 4])
    cbias = cload("cbias", cb_d, [128, 24])
    dcol = cload("dcol", dcol_d, [128, 16])
    ng = cload("ng", ng_d, [128, 16])
    tm = cload("tm", tm_d, [128, 256])
    cm = cload("cm", cm_d, [128, 384])
    onesf = P.sb("onesf", [128, 128], F32)
    onesb = P.sb("onesb", [128, 128], BF16)
    P.op("dve", lambda e: e.memset(onesf[:], 1.0), w=["onesf"])
    P.op("dve", lambda e: e.memset(onesb[:], 1.0), w=["onesb"])
    P.op("act", lambda e: e.activation(out=aneg[:], in_=aneg[:], func=AF.Exp), r=["aneg"], w=["aneg"])
    P.op("dve", lambda e: e.tensor_scalar(out=aneg[:], in0=aneg[:], scalar1=-1.0, scalar2=None, op0=ALU.mult),
         r=["aneg"], w=["aneg"])
    wdt = P.sb("wdt", [128, 16, 32], BF16)
    P.dma("pool", wdt[:], w_d[:, 5120:5152].rearrange("(c p) n -> p c n", p=128), w=["wdt"], sem="wdt")

    xt = P.sb("xt", [128, D], F32)
    hn_t = P.sb("hn", [128, D], F32)
    rs_t = P.sb("rs", [128, 1], F32)
    hT = P.sb("hT", [128, 16, L], BF16)
    sz = P.sb("sz", [128, 16, L], F32)
    xc = P.sb("xc", [128, 16, L], F32)
    BT = P.sb("BT", [128, 4, L], BF16)
    CT = P.sb("CT", [128, 4, L], BF16)
    ue = [P.sb(f"ue{i}", [128, L + 3], F32) for i in range(2)]
    acc = [P.sb(f"acc{i}", [128, L], F32) for i in range(2)]
    halo = P.sb("halo", [128, 24, 3], F32)
    P.op("dve", lambda e: e.memset(halo[:], 0.0), w=["halo"])
    dt_t = P.sb("dt", [128, 2, 32], F32)
    a_t = P.sb("a", [128, 2, 32], F32)
    acum = P.sb("acum", [128, 2, 32], F32)
    acl = P.sb("acl", [128, 32], F32)
    dA = P.sb("dA", [128, 32], F32)
    w_t = P.sb("w", [128, 2, 32], F32)
    acumT = P.sb("acumT", [32, L], F32)
    xdtp = P.sb("xdtp", [128, 2, 16, 2, 128], BF16)
    P.op("dve", lambda e: e.memset(xdtp[:], 0.0), w=["xdtp"])
    xdtw = P.sb("xdtw", [128, 2, 2048], BF16)
    Btm = P.sb("Btm", [128, 2, 512], BF16)
    cbm = P.sb("cbm", [128, 4, 384], F32)
    selb = [P.sb(f"selb{i}", [32, 128], F32) for i in range(2)]
    selp = P.sb("selp", [32, 128], F32)
    seg = [P.sb(f"seg{i}", [128, 384], F32) for i in range(2)]
    Mh = [P.sb(f"Mh{i}", [128, 384], BF16) for i in range(2)]
    E_t = P.sb("E", [128, L], F32)
    t_t = P.sb("tt", [128, L], F32)
    y_t = P.sb("yy", [128, L], F32)
    yg = P.sb("yg", [128, 4, L], F32)
    ysq = P.sb("ysq", [128, L], BF16)
    rsg = P.sb("rsg", [128, L], F32)
    yo = [P.sb(f"yo{i}", [128, L], BF16) for i in range(2)]
    state = P.sb("state", [128, 2048], F32)
    stateb = P.sb("stateb", [128, 2048], BF16)
    P.op("dve", lambda e: e.memset(state[:], 0.0), w=["state"])
    P.op("dve", lambda e: e.memset(stateb[:], 0.0), w=["stateb"])
    outs = []
    ps = C.ps
    cpi = [0]

    def evac(src, dst, rkeys, wkeys, func=AF.Copy):
        cpi[0] += 1
        if cpi[0] % 2 == 0 or func != AF.Copy:
            P.op("act", lambda e: e.activation(out=dst, in_=src, func=func), r=rkeys, w=wkeys)
        else:
            P.op("dve", lambda e: e.tensor_copy(out=dst, in_=src), r=rkeys, w=wkeys)

    yoi = 0
    for ci in range(NCK):
        t0 = ci * L
        for i in range(2):
            P.dma("sp", xt[:], x2[t0 + i * 128:t0 + (i + 1) * 128, :], w=["xt"], sem="xt")
            C.rstd(xt[:], "xt", D, hn_t[:], "hn", rs_t, "rs")
            P.op("dve", lambda e: e.scalar_tensor_tensor(out=hn_t[:], in0=xt[:], scalar=rs_t[:], in1=gb[:],
                                                         op0=ALU.mult, op1=ALU.mult), r=["xt", "rs", "gb"], w=["hn"])
            C.transpose_to(hn_t, "hn", 16, lambda k0, n, i=i: hT[:, k0:k0 + n, i * 128:(i + 1) * 128], "hT", [0, 1])
        for i in range(2):
            for kc in range(16):
                P.op("pe", lambda e, kc=kc, i=i: e.matmul(out=ps[2][:, 0:32], lhsT=hT[:, kc, i * 128:(i + 1) * 128],
                                                          rhs=wdt[:, kc, :], start=(kc == 0), stop=(kc == 15)),
                     r=["hT", "wdt"], w=["ps2"])
            P.op("dve", lambda e, i=i: e.tensor_tensor(out=dt_t[:, i, :], in0=ps[2][:, 0:32], in1=dtb[:], op=ALU.add),
                 r=["ps2", "dtbb"], w=["dt"])
            P.op("act", lambda e, i=i: e.activation(out=dt_t[:, i, :], in_=dt_t[:, i, :], func=AF.Exp), r=["dt"], w=["dt"])
            P.op("act", lambda e, i=i: e.activation(out=dt_t[:, i, :], in_=dt_t[:, i, :], func=AF.Ln, bias=1.0),
                 r=["dt"], w=["dt"])
            P.op("dve", lambda e, i=i: e.tensor_tensor(out=a_t[:, i, :], in0=dt_t[:, i, :], in1=aneg[:], op=ALU.mult),
                 r=["dt", "aneg"], w=["a"])
        P.op("pe", lambda e: e.matmul(out=ps[2][:, 0:32], lhsT=tm[:, 0:128], rhs=a_t[:, 0, :], start=True, stop=True),
             r=["tm", "a"], w=["ps2"])
        P.op("dve", lambda e: e.tensor_copy(out=acum[:, 0, :], in_=ps[2][:, 0:32]), r=["ps2"], w=["acum"])
        P.op("pe", lambda e: e.matmul(out=ps[2][:, 0:32], lhsT=onesf[:], rhs=a_t[:, 0, :], start=True, stop=False),
             r=["onesf", "a"], w=["ps2"])
        P.op("pe", lambda e: e.matmul(out=ps[2][:, 0:32], lhsT=tm[:, 0:128], rhs=a_t[:, 1, :], start=False, stop=True),
             r=["tm", "a"], w=["ps2"])
        P.op("dve", lambda e: e.tensor_copy(out=acum[:, 1, :], in_=ps[2][:, 0:32]), r=["ps2"], w=["acum"])
        P.op("pe", lambda e: e.matmul(out=ps[2][:, 0:32], lhsT=onesf[:], rhs=a_t[:, 0, :], start=True, stop=False),
             r=["onesf", "a"], w=["ps2"])
        P.op("pe", lambda e: e.matmul(out=ps[2][:, 0:32], lhsT=onesf[:], rhs=a_t[:, 1, :], start=False, stop=True),
             r=["onesf", "a"], w=["ps2"])
        P.op("dve", lambda e: e.tensor_copy(out=acl[:], in_=ps[2][:, 0:32]), r=["ps2"], w=["acl"])
        P.op("act", lambda e: e.activation(out=dA[:], in_=acl[:], func=AF.Exp), r=["acl"], w=["dA"])
        for i in range(2):
            P.op("dve", lambda e, i=i: e.tensor_tensor(out=w_t[:, i, :], in0=acl[:], in1=acum[:, i, :], op=ALU.subtract),
                 r=["acl", "acum"], w=["w"])
            P.op("act", lambda e, i=i: e.activation(out=w_t[:, i, :], in_=w_t[:, i, :], func=AF.Exp), r=["w"], w=["w"])
            P.op("dve", lambda e, i=i: e.tensor_tensor(out=w_t[:, i, :], in0=w_t[:, i, :], in1=dt_t[:, i, :], op=ALU.mult),
                 r=["w", "dt"], w=["w"])
            P.op("pe", lambda e, i=i: e.transpose(out=ps[2][0:32, i * 128:(i + 1) * 128], in_=acum[:, i, :],
                                                  identity=C.identf[:]), r=["acum", "identf"], w=["ps2"])
        P.op("dve", lambda e: e.tensor_copy(out=acumT[:], in_=ps[2][0:32, 0:L]), r=["ps2"], w=["acumT"])
        ipi = 0
        for blk in range(10):
            kw, vw = C.wload(w_d[:, blk * 512:(blk + 1) * 512].rearrange("(c p) n -> p c n", p=128))
            for j in range(4):
                oc = blk * 4 + j
                bk = 3 + (ipi % 2); ipi += 1
                for kc in range(16):
                    P.op("pe", lambda e, kc=kc, j=j, bk=bk, vw=vw: e.matmul(
                        out=ps[bk][:, 0:L], lhsT=vw[:, kc, j * 128:(j + 1) * 128], rhs=hT[:, kc, :],
                        start=(kc == 0), stop=(kc == 15)), r=[kw, "hT"], w=[f"ps{bk}"])
                if oc < 16:
                    P.op("act", lambda e, oc=oc, bk=bk: e.activation(out=sz[:, oc, :], in_=ps[bk][:, 0:L], func=AF.Silu),
                         r=[f"ps{bk}"], w=["sz"])
                    continue
                cc = oc - 16
                ub = cc % 2
                uk = f"ue{ub}"
                ak = f"acc{ub}"
                P.op("dve", lambda e, ub=ub, cc=cc: e.tensor_copy(out=ue[ub][:, 0:3], in_=halo[:, cc, :]),
                     r=["halo"], w=[uk])
                evac(ps[bk][:, 0:L], ue[ub][:, 3:3 + L], [f"ps{bk}"], [uk])
                P.op("dve", lambda e, ub=ub, cc=cc: e.tensor_copy(out=halo[:, cc, :], in_=ue[ub][:, L:L + 3]),
                     r=[uk], w=["halo"])
                P.op("dve", lambda e, ub=ub, cc=cc: e.tensor_scalar(
                    out=acc[ub][:], in0=ue[ub][:, 0:L], scalar1=cw[:, cc, 0:1], scalar2=cbias[:, cc:cc + 1],
                    op0=ALU.mult, op1=ALU.add), r=[uk, "cw", "cbias"], w=[ak])
                for jt in range(1, 4):
                    P.op("dve", lambda e, ub=ub, cc=cc, jt=jt: e.scalar_tensor_tensor(
                        out=acc[ub][:], in0=ue[ub][:, jt:jt + L], scalar=cw[:, cc, jt:jt + 1], in1=acc[ub][:],
                        op0=ALU.mult, op1=ALU.add), r=[uk, "cw", ak], w=[ak])
                if cc < 16:
                    dst, dk = xc[:, cc, :], "xc"
                elif cc < 20:
                    dst, dk = BT[:, cc - 16, :], "BT"
                else:
                    dst, dk = CT[:, cc - 20, :], "CT"
                P.op("act", lambda e, ub=ub, dst=dst: e.activation(out=dst, in_=acc[ub][:], func=AF.Silu),
                     r=[ak], w=[dk])
        for i in range(2):
            for q in range(4):
                bk = q % 2
                for j in range(4):
                    cc = q * 4 + j
                    P.op("pe", lambda e, cc=cc, j=j, i=i, bk=bk: e.transpose(
                        out=ps[bk][:, j * 128:(j + 1) * 128], in_=xc[:, cc, i * 128:(i + 1) * 128], identity=C.identf[:]),
                         r=["xc", "identf"], w=[f"ps{bk}"])
                src8 = ps[bk][:].rearrange("p (h d) -> p h d", h=8)
                P.op("dve", lambda e, i=i, q=q, src8=src8: e.tensor_tensor(
                    out=xdtw[:, i, q * 512:(q + 1) * 512].rearrange("p (h d) -> p h d", h=8), in0=src8,
                    in1=w_t[:, i, q * 8:(q + 1) * 8].unsqueeze(2).to_broadcast([128, 8, 64]), op=ALU.mult),
                     r=[f"ps{bk}", "w"], w=["xdtw"])
                src42 = ps[bk][:].rearrange("p (c e d) -> p c e d", c=4, e=2)
                dt42 = dt_t[:, i, q * 8:(q + 1) * 8].rearrange("p (c e) -> p c e", e=2)
                for e_ in range(2):
                    P.op("dve", lambda e, i=i, q=q, e_=e_, src42=src42, dt42=dt42: e.tensor_tensor(
                        out=xdtp[:, i, q * 4:(q + 1) * 4, e_, e_ * 64:(e_ + 1) * 64], in0=src42[:, :, e_, :],
                        in1=dt42[:, :, e_].unsqueeze(2).to_broadcast([128, 4, 64]), op=ALU.mult),
                         r=[f"ps{bk}", "dt"], w=["xdtp"])
            pvb = ps[2][:].bitcast(BF16)
            for g in range(4):
                P.op("pe", lambda e, g=g, i=i, pvb=pvb: e.transpose(
                    out=pvb[:, g * 128:(g + 1) * 128], in_=BT[:, g, i * 128:(i + 1) * 128], identity=C.identb[:]),
                     r=["BT", "identb"], w=["ps2"])
            evac(pvb[:, 0:512], Btm[:, i, :], ["ps2"], ["Btm"])
        for g in range(4):
            P.op("pe", lambda e, g=g: e.matmul(out=ps[3][:, 0:L], lhsT=BT[:, g, 0:128], rhs=CT[:, g, 0:L],
                                               start=True, stop=True), r=["BT", "CT"], w=["ps3"])
            P.op("pe", lambda e, g=g: e.matmul(out=ps[4][:, 0:128], lhsT=BT[:, g, 128:256], rhs=CT[:, g, 128:256],
                                               start=True, stop=True), r=["BT", "CT"], w=["ps4"])
            P.op("dve", lambda e, g=g: e.tensor_tensor(out=cbm[:, g, 0:256], in0=ps[3][:, 0:L], in1=cm[:, 0:256],
                                                       op=ALU.mult), r=["ps3", "cm"], w=["cbm"])
            P.op("dve", lambda e, g=g: e.tensor_tensor(out=cbm[:, g, 256:384], in0=ps[4][:, 0:128], in1=cm[:, 256:384],
                                                       op=ALU.mult), r=["ps4", "cm"], w=["cbm"])
        for c in range(NCH):
            g = c // 4
            for e_ in range(2):
                h = 2 * c + e_
                sb_ = h % 2
                P.op("dve", lambda e, h=h, sb_=sb_: e.tensor_copy(
                    out=selb[sb_][:], in_=C.identf[0:32, h:h + 1].to_broadcast([32, 128])),
                     r=["identf"], w=[f"selb{sb_}"])
                pa = 5 + sb_
                P.op("pe", lambda e, sb_=sb_, pa=pa: e.matmul(out=ps[pa][:, 0:L], lhsT=selb[sb_][:], rhs=acumT[:],
                                                              start=True, stop=True),
                     r=[f"selb{sb_}", "acumT"], w=[f"ps{pa}"])
                P.op("dve", lambda e, h=h, sb_=sb_, pa=pa: e.tensor_scalar(
                    out=seg[sb_][:, 0:256], in0=ps[pa][:, 0:L], scalar1=acum[:, 0, h:h + 1], scalar2=0.0,
                    op0=ALU.subtract, op1=ALU.min), r=[f"ps{pa}", "acum"], w=[f"seg{sb_}"])
                P.op("dve", lambda e, h=h, sb_=sb_, pa=pa: e.tensor_scalar(
                    out=seg[sb_][:, 256:384], in0=ps[pa][:, 128:256], scalar1=acum[:, 1, h:h + 1], scalar2=0.0,
                    op0=ALU.subtract, op1=ALU.min), r=[f"ps{pa}", "acum"], w=[f"seg{sb_}"])
                P.op("act", lambda e, sb_=sb_: e.activation(out=seg[sb_][:], in_=seg[sb_][:], func=AF.Exp),
                     r=[f"seg{sb_}"], w=[f"seg{sb_}"])
                P.op("pool", lambda e, sb_=sb_, g=g: e.tensor_tensor(out=Mh[sb_][:], in0=seg[sb_][:], in1=cbm[:, g, :],
                                                                    op=ALU.mult),
                     r=[f"seg{sb_}", "cbm"], w=[f"Mh{sb_}"])
            P.op("dve", lambda e, c=c: e.tensor_copy(out=selp[:, 0:64], in_=C.identf[0:32, 2 * c:2 * c + 1].to_broadcast([32, 64])),
                 r=["identf"], w=["selp"])
            P.op("dve", lambda e, c=c: e.tensor_copy(out=selp[:, 64:128],
                                                     in_=C.identf[0:32, 2 * c + 1:2 * c + 2].to_broadcast([32, 64])),
                 r=["identf"], w=["selp"])
            P.op("pe", lambda e: e.matmul(out=ps[7][:, 0:L], lhsT=selp[:], rhs=acumT[:], start=True, stop=True),
                 r=["selp", "acumT"], w=["ps7"])
            P.op("act", lambda e: e.activation(out=E_t[:], in_=ps[7][:, 0:L], func=AF.Exp), r=["ps7"], w=["E"])
            P.op("pe", lambda e, c=c: e.matmul(out=ps[0][:, 0:L], lhsT=xdtp[:, 0, c, 0, :], rhs=Mh[0][:, 0:256],
                                               start=True, stop=False), r=["xdtp", "Mh0"], w=["ps0"])
            P.op("pe", lambda e, c=c: e.matmul(out=ps[0][:, 0:L], lhsT=xdtp[:, 0, c, 1, :], rhs=Mh[1][:, 0:256],
                                               start=False, stop=False), r=["xdtp", "Mh1"], w=["ps0"])
            P.op("pe", lambda e, c=c: e.matmul(out=ps[0][:, 128:256], lhsT=xdtp[:, 1, c, 0, :], rhs=Mh[0][:, 256:384],
                                               start=False, stop=False), r=["xdtp", "Mh0"], w=["ps0"])
            P.op("pe", lambda e, c=c: e.matmul(out=ps[0][:, 128:256], lhsT=xdtp[:, 1, c, 1, :], rhs=Mh[1][:, 256:384],
                                               start=False, stop=True), r=["xdtp", "Mh1"], w=["ps0"])
            P.op("pe", lambda e, c=c, g=g: e.matmul(out=ps[1][:, 0:L], lhsT=stateb[:, c * 128:(c + 1) * 128],
                                                    rhs=CT[:, g, :], start=True, stop=True),
                 r=["stateb", "CT"], w=["ps1"])
            P.op("dve", lambda e: e.tensor_tensor(out=t_t[:], in0=ps[1][:, 0:L], in1=E_t[:], op=ALU.mult),
                 r=["ps1", "E"], w=["tt"])
            P.op("dve", lambda e: e.tensor_tensor(out=y_t[:], in0=ps[0][:, 0:L], in1=t_t[:], op=ALU.add),
                 r=["ps0", "tt"], w=["yy"])
            P.op("dve", lambda e, c=c: e.scalar_tensor_tensor(out=y_t[:], in0=xc[:, c, :], scalar=dcol[:, c:c + 1],
                                                              in1=y_t[:], op0=ALU.mult, op1=ALU.add),
                 r=["xc", "dcol", "yy"], w=["yy"])
            P.op("pool", lambda e, c=c: e.tensor_tensor(out=yg[:, c % 4, :], in0=y_t[:], in1=sz[:, c, :], op=ALU.mult),
                 r=["yy", "sz"], w=["yg"])
            P.op("act", lambda e, c=c: e.activation(out=ysq[:], in_=yg[:, c % 4, :], func=AF.Square),
                 r=["yg"], w=["ysq"])
            P.op("pe", lambda e, c=c: e.matmul(out=ps[2][:, 0:L], lhsT=onesb[:], rhs=ysq[:], start=(c % 4 == 0),
                                               stop=(c % 4 == 3)), r=["onesb", "ysq"], w=["ps2"])
            if c % 4 == 3:
                P.op("act", lambda e: e.activation(out=rsg[:], in_=ps[2][:, 0:L], func=AF.Sqrt, scale=1.0 / 512,
                                                   bias=C.epsb[:]), r=["ps2", "epsb"], w=["rsg"])
                P.op("dve", lambda e: e.reciprocal(out=rsg[:], in_=rsg[:]), r=["rsg"], w=["rsg"])
                for k in range(4):
                    cc = g * 4 + k
                    ob = yoi % 2; yoi += 1
                    P.op("dve", lambda e, k=k, cc=cc, ob=ob: e.scalar_tensor_tensor(
                        out=yo[ob][:], in0=yg[:, k, :], scalar=ng[:, cc:cc + 1], in1=rsg[:], op0=ALU.mult, op1=ALU.mult),
                         r=["yg", "ng", "rsg"], w=[f"yo{ob}"])
                    outs.append(P.dma("sp", y_d[cc * 128:(cc + 1) * 128, t0:t0 + L], yo[ob][:], r=[f"yo{ob}"],
                                      sem=f"yo{ob}"))
        for g in range(4):
            bk = 3 + g % 2
            for i in range(2):
                P.op("pe", lambda e, g=g, i=i, bk=bk: e.matmul(
                    out=ps[bk][:], lhsT=Btm[:, i, g * 128:(g + 1) * 128], rhs=xdtw[:, i, g * 512:(g + 1) * 512],
                    start=(i == 0), stop=(i == 1)), r=["Btm", "xdtw"], w=[f"ps{bk}"])
            sv = state[:, g * 512:(g + 1) * 512].rearrange("p (h d) -> p h d", h=8)
            P.op("dve", lambda e, g=g, sv=sv: e.tensor_tensor(
                out=sv, in0=sv, in1=dA[:, g * 8:(g + 1) * 8].unsqueeze(2).to_broadcast([128, 8, 64]), op=ALU.mult),
                 r=["state", "dA"], w=["state"])
            P.op("dve", lambda e, g=g, bk=bk: e.tensor_tensor(
                out=state[:, g * 512:(g + 1) * 512], in0=ps[bk][:], in1=state[:, g * 512:(g + 1) * 512], op=ALU.add),
                 r=[f"ps{bk}", "state"], w=["state"])
            P.op("act", lambda e, g=g: e.activation(out=stateb[:, g * 512:(g + 1) * 512],
                                                    in_=state[:, g * 512:(g + 1) * 512], func=AF.Copy),
                 r=["state"], w=["stateb"])
    nc = P.emit(final_wait_ops=outs)
    return C, nc


CAP = 896
NE = 8
NST = CAP // 128


class Arena:
    def __init__(self, P, nbytes):
        self.t = P.sb("arena", [128, nbytes // 2], BF16)
        self.off = 0
        self.nbytes = nbytes

    def reset(self):
        self.off = 0

    def view(self, shape, dtype, parts=128):
        esz = 4 if dtype in (F32, I32) else 2
        n = 1
        for s in shape[1:]:
            n *= s
        nb = n * esz
        assert self.off + nb <= self.nbytes, (self.off, nb, self.nbytes)
        v = self.t[0:parts, self.off // 2:(self.off + nb) // 2]
        self.off += nb
        if esz == 4:
            v = v.bitcast(dtype)
        if len(shape) == 3:
            v = v.rearrange("p (a b) -> p a b", a=shape[1])
        return v


def build_l4(T=2048, TB=512, KO=4096, experts=NE):
    C = Ctx(nring=7, ring_elems=4096)
    P = C.P
    NT = T // 128
    x2 = P.dram("x2", [T, D], F32, kind="ExternalInput")
    yT = P.dram("yT", [KO, T], BF16, kind="ExternalInput")
    w_o = P.dram("w_o", [KO, D], F32, kind="ExternalInput")
    g_d = P.dram("g", [1, D], F32, kind="ExternalInput")
    gf_d = P.dram("gfin", [1, D], F32, kind="ExternalInput")
    rt_d = P.dram("router", [D, NE], F32, kind="ExternalInput")
    wg = P.dram("wg", [NE, D, FF], F32, kind="ExternalInput")
    wu = P.dram("wu", [NE, D, FF], F32, kind="ExternalInput")
    wd = P.dram("wd", [NE, FF, D], F32, kind="ExternalInput")
    iota_d = P.dram("c_iota", [128, CAP], F32, kind="ExternalInput")
    tris_d = P.dram("c_tris", [128, 128], BF16, kind="ExternalInput")
    out = P.dram("out", [T, D], F32, kind="ExternalOutput")
    x3d = P.dram("x3acc", [T, D], F32)
    hnbd = P.dram("hnb", [T, D], BF16)

    gb = P.sb("gb", [128, D], F32)
    P.dma("sp", gb[:], g_d.partition_broadcast(128), w=["gb"], sem="gbl")
    rsb = P.sb("rsb", [128, 16, NE], F32)
    P.dma("sp", rsb[:], rt_d.rearrange("(c p) n -> p c n", p=128), w=["rsb"], sem="G:consts")
    iota = P.sb("iota", [128, CAP], F32)
    P.dma("sp", iota[:], iota_d, w=["iota"], sem="G:consts")
    tris = P.sb("tris", [128, 128], BF16)
    P.dma("sp", tris[:], tris_d, w=["tris"], sem="G:consts")
    onesb = P.sb("onesb", [128, 128], BF16)
    P.op("dve", lambda e: e.memset(onesb[:], 1.0), w=["onesb"])
    gates = P.sb("gates", [128, NT, NE], F32)
    self_f = P.sb("self", [128, NT, NE], F32)
    selb = P.sb("selbb", [128, NT, NE], BF16)
    dest = P.sb("dest", [128, NT, NE], F32)
    rs_t = P.sb("rs", [128, 1], F32)
    sm = {n: P.sb("sm_" + n, [128, NE], F32) for n in ["lg", "m1k", "l2", "m2k", "t"]}
    sc1 = {n: P.sb("sc_" + n, [128, 1], F32) for n in ["m1", "m2", "d", "g1", "g2"]}

    A = Arena(P, 122 * 1024)
    ps = C.ps
    cpi = [0]

    def evac(src, dst, rkeys, wkeys):
        cpi[0] += 1
        if cpi[0] % 2 == 0:
            P.op("act", lambda e: e.activation(out=dst, in_=src, func=AF.Copy), r=rkeys, w=wkeys)
        else:
            P.op("dve", lambda e: e.tensor_copy(out=dst, in_=src), r=rkeys, w=wkeys)

    ntt = TB // 128
    KC = KO // 128
    xb = A.view([128, ntt, D], F32)
    yTb = A.view([128, KC, TB], BF16)
    hn_t = A.view([128, D], F32)
    hnb = [A.view([128, D], BF16) for _ in range(2)]
    hTf = A.view([128, 16, 128], F32)
    wcb = 4096 // KC
    hi = 0
    for tb in range(T // TB):
        t0 = tb * TB
        for tt in range(ntt):
            P.dma("sp", xb[:, tt, :], x2[t0 + tt * 128:t0 + (tt + 1) * 128, :], w=[f"xb{tt}"], sem=f"xb{tt}")
        P.dma("sp", yTb, yT[:, t0:t0 + TB].rearrange("(c p) t -> p c t", p=128), w=["yTb"], sem="yTb")
        di = 0
        for cb in range(D // wcb):
            kw, vw = C.wload(w_o[:, cb * wcb:(cb + 1) * wcb].rearrange("(c p) n -> p c n", p=128))
            for tt in range(ntt):
                bd = di % 2
                di += 1
                for kc in range(KC):
                    P.op("pe", lambda e, kc=kc, tt=tt, bd=bd, vw=vw: e.matmul(
                        out=ps[bd][:, 0:wcb], lhsT=yTb[:, kc, tt * 128:(tt + 1) * 128], rhs=vw[:, kc, :],
                        start=(kc == 0), stop=(kc == KC - 1)), r=["yTb", kw], w=[f"ps{bd}"])
                P.op("dve", lambda e, tt=tt, cb=cb, bd=bd: e.tensor_tensor(
                    out=xb[:, tt, cb * wcb:(cb + 1) * wcb], in0=ps[bd][:, 0:wcb],
                    in1=xb[:, tt, cb * wcb:(cb + 1) * wcb], op=ALU.add), r=[f"ps{bd}", f"xb{tt}"], w=[f"xb{tt}"])
        for tt in range(ntt):
            i = tb * ntt + tt
            xk = f"xb{tt}"
            P.dma("sp", x3d[i * 128:(i + 1) * 128, :], xb[:, tt, :], r=[xk], w=[f"x3d{i}"], sem=f"x3o{tt}")
            C.rstd(xb[:, tt, :], xk, D, hn_t, "hn", rs_t, "rs")
            P.op("dve", lambda e, tt=tt: e.scalar_tensor_tensor(out=hn_t, in0=xb[:, tt, :], scalar=rs_t[:], in1=gb[:],
                                                                op0=ALU.mult, op1=ALU.mult),
                 r=[xk, "rs", "gb"], w=["hn"])
            hb_ = hi % 2; hi += 1
            P.op("act", lambda e, hb_=hb_: e.activation(out=hnb[hb_], in_=hn_t, func=AF.Copy), r=["hn"], w=[f"hnb{hb_}"])
            P.dma("sp", hnbd[i * 128:(i + 1) * 128, :], hnb[hb_], r=[f"hnb{hb_}"], w=[f"hnbd{i}"], sem=f"hnbo{hb_}")
            for q in range(4):
                bk = 2 + q % 2
                for j in range(4):
                    c = q * 4 + j
                    P.op("pe", lambda e, c=c, j=j, bk=bk: e.transpose(out=ps[bk][:, j * 128:(j + 1) * 128],
                                                                      in_=hn_t[:, c * 128:(c + 1) * 128],
                                                                      identity=C.identf[:]),
                         r=["hn", "identf"], w=[f"ps{bk}"])
                evac(ps[bk][:].rearrange("p (c t) -> p c t", c=4), hTf[:, q * 4:(q + 1) * 4, :], [f"ps{bk}"], ["hTf"])
            for kc in range(16):
                P.op("pe", lambda e, kc=kc: e.matmul(out=ps[4][:, 0:NE], lhsT=hTf[:, kc, :], rhs=rsb[:, kc, :],
                                                     start=(kc == 0), stop=(kc == 15)), r=["hTf", "rsb"], w=["ps4"])
            lg, m1k, l2, m2k, tmp = sm["lg"], sm["m1k"], sm["l2"], sm["m2k"], sm["t"]
            m1, m2, dd, g1, g2 = sc1["m1"], sc1["m2"], sc1["d"], sc1["g1"], sc1["g2"]
            P.op("dve", lambda e: e.tensor_copy(out=lg[:], in_=ps[4][:, 0:NE]), r=["ps4"], w=["sm_lg"])
            P.op("dve", lambda e: e.reduce_max(out=m1[:], in_=lg[:], axis=AX.X), r=["sm_lg"], w=["sc_m1"])
            P.op("dve", lambda e: e.tensor_scalar(out=m1k[:], in0=lg[:], scalar1=m1[:], scalar2=None, op0=ALU.is_equal),
                 r=["sm_lg", "sc_m1"], w=["sm_m1k"])
            P.op("dve", lambda e: e.scalar_tensor_tensor(out=l2[:], in0=m1k[:], scalar=-1e30, in1=lg[:], op0=ALU.mult,
                                                         op1=ALU.add), r=["sm_m1k", "sm_lg"], w=["sm_l2"])
            P.op("dve", lambda e: e.reduce_max(out=m2[:], in_=l2[:], axis=AX.X), r=["sm_l2"], w=["sc_m2"])
            P.op("dve", lambda e: e.tensor_scalar(out=m2k[:], in0=l2[:], scalar1=m2[:], scalar2=None, op0=ALU.is_equal),
                 r=["sm_l2", "sc_m2"], w=["sm_m2k"])
            P.op("dve", lambda e: e.tensor_tensor(out=dd[:], in0=m2[:], in1=m1[:], op=ALU.subtract),
                 r=["sc_m1", "sc_m2"], w=["sc_d"])
            P.op("act", lambda e: e.activation(out=g1[:], in_=dd[:], func=AF.Exp), r=["sc_d"], w=["sc_g1"])
            P.op("dve", lambda e: e.tensor_scalar(out=g1[:], in0=g1[:], scalar1=1.0, scalar2=None, op0=ALU.add),
                 r=["sc_g1"], w=["sc_g1"])
            P.op("dve", lambda e: e.reciprocal(out=g1[:], in_=g1[:]), r=["sc_g1"], w=["sc_g1"])
            P.op("dve", lambda e: e.tensor_scalar(out=g2[:], in0=g1[:], scalar1=-1.0, scalar2=1.0, op0=ALU.mult,
                                                  op1=ALU.add), r=["sc_g1"], w=["sc_g2"])
            P.op("dve", lambda e: e.tensor_scalar(out=tmp[:], in0=m1k[:], scalar1=g1[:], scalar2=None, op0=ALU.mult),
                 r=["sm_m1k", "sc_g1"], w=["sm_t"])
            P.op("dve", lambda e, i=i: e.scalar_tensor_tensor(out=gates[:, i, :], in0=m2k[:], scalar=g2[:], in1=tmp[:],
                                                              op0=ALU.mult, op1=ALU.add),
                 r=["sm_m2k", "sc_g2", "sm_t"], w=["gates"])
            P.op("dve", lambda e, i=i: e.tensor_tensor(out=self_f[:, i, :], in0=m1k[:], in1=m2k[:], op=ALU.add),
                 r=["sm_m1k", "sm_m2k"], w=["self"])
            P.op("dve", lambda e, i=i: e.tensor_copy(out=selb[:, i, :], in_=self_f[:, i, :]), r=["self"], w=["selbb"])
    for i in range(NT):
        for ip in range(i):
            P.op("pe", lambda e, ip=ip, i=i: e.matmul(out=ps[4][:, 0:NE], lhsT=onesb[:], rhs=selb[:, ip, :],
                                                      start=(ip == 0), stop=False), r=["onesb", "selbb"], w=["ps4"])
        P.op("pe", lambda e, i=i: e.matmul(out=ps[4][:, 0:NE], lhsT=tris[:], rhs=selb[:, i, :], start=(i == 0), stop=True),
             r=["tris", "selbb"], w=["ps4"])
        P.op("dve", lambda e, i=i: e.tensor_copy(out=dest[:, i, :], in_=ps[4][:, 0:NE]), r=["ps4"], w=["dest"])

    P.barrier()
    A.reset()
    hT_e = A.view([128, 16, CAP], BF16)
    A.off = 0
    yEb = A.view([128, NST, D], BF16)
    yE = A.view([128, NST, D], F32)
    Se = [A.view([128, CAP], BF16) for _ in range(2)]
    hTM = [A.view([128, 4, 512], BF16) for _ in range(2)]
    a_bufs = [A.view([128, 2, CAP], BF16) for _ in range(2)]
    sg_bufs = [A.view([128, 512], F32) for _ in range(2)]
    SeT = [A.view([128, NST, 128], BF16) for _ in range(2)]
    acc_t = A.view([128, D], F32)
    HALF = CAP // 2
    sei = 0

    def make_se(e_, i):
        nonlocal sei
        b = sei % 2; sei += 1
        P.op("dve", lambda e, b=b, i=i, e_=e_: e.tensor_scalar(
            out=Se[b], in0=iota[:], scalar1=dest[:, i, e_:e_ + 1], scalar2=self_f[:, i, e_:e_ + 1],
            op0=ALU.is_equal, op1=ALU.mult), r=["iota", "dest", "self"], w=[f"Se{b}"])
        return b

    hmi = 0
    for e_ in range(experts):
        for kg in range(4):
            for i in range(NT):
                b = make_se(e_, i)
                if i % 4 == 0:
                    hb_ = hmi % 2; hmi += 1
                    P.dma("sp", hTM[hb_], hnbd[i * 128:(i + 4) * 128, kg * 512:(kg + 1) * 512].rearrange("(a p) k -> p a k", p=128),
                          r=[f"hnbd{i + q}" for q in range(4)], w=[f"hTM{hb_}"], sem=f"hTM{hb_}")
                for kc4 in range(4):
                    for sc in range(2):
                        bk = kc4 * 2 + sc
                        P.op("pe", lambda e, hb_=hb_, b=b, kc4=kc4, sc=sc, bk=bk, i=i: e.matmul(
                            out=ps[bk][:, 0:HALF], lhsT=hTM[hb_][:, i % 4, kc4 * 128:(kc4 + 1) * 128],
                            rhs=Se[b][:, sc * HALF:(sc + 1) * HALF], start=(i == 0), stop=(i == NT - 1)),
                             r=[f"hTM{hb_}", f"Se{b}"], w=[f"ps{bk}"])
            for kc4 in range(4):
                for sc in range(2):
                    bk = kc4 * 2 + sc
                    evac(ps[bk][:, 0:HALF], hT_e[:, kg * 4 + kc4, sc * HALF:(sc + 1) * HALF], [f"ps{bk}"], ["hT_e"])
        P.op("pool", lambda e: e.memset(yE, 0.0), w=["yE"])
        ffn_core(C, hT_e, "hT_e", CAP, [(st * 128, 128) for st in range(NST)], wg[e_], wu[e_], wd[e_],
                 lambda ti, fbk: (yE[:, ti, fbk * 512:(fbk + 1) * 512], "yE"),
                 psG=[4, 5], psU=[6, 7], psD=[0, 1, 2, 3], a_bufs=a_bufs, sg_bufs=sg_bufs, HB=256,
                 akeys=["ab0", "ab1"], sgkeys=["sgb0", "sgb1"])
        for st in range(NST):
            evac(yE[:, st, :], yEb[:, st, :], ["yE"], ["hT_e"])
        for i in range(NT):
            b = make_se(e_, i)
            tb_ = i % 2
            pvb = ps[4 + tb_][:].bitcast(BF16)
            for st in range(NST):
                P.op("pe", lambda e, b=b, st=st, pvb=pvb: e.transpose(out=pvb[:, st * 128:(st + 1) * 128],
                                                                      in_=Se[b][:, st * 128:(st + 1) * 128],
                                                                      identity=C.identb[:]),
                     r=[f"Se{b}", "identb"], w=[f"ps{4 + tb_}"])
            evac(pvb[:, 0:CAP].rearrange("p (s t) -> p s t", s=NST), SeT[tb_], [f"ps{4 + tb_}"], [f"SeT{tb_}"])
            P.dma("sp", acc_t, x3d[i * 128:(i + 1) * 128, :], r=[f"x3d{i}"], w=["acc_t"], sem="accl")
            for fbk in range(4):
                bk = fbk
                for st in range(NST):
                    P.op("pe", lambda e, tb_=tb_, st=st, fbk=fbk, bk=bk: e.matmul(
                        out=ps[bk][:], lhsT=SeT[tb_][:, st, :], rhs=yEb[:, st, fbk * 512:(fbk + 1) * 512],
                        start=(st == 0), stop=(st == NST - 1)), r=[f"SeT{tb_}", "hT_e"], w=[f"ps{bk}"])
                P.op("dve", lambda e, fbk=fbk, bk=bk, i=i, e_=e_: e.scalar_tensor_tensor(
                    out=acc_t[:, fbk * 512:(fbk + 1) * 512], in0=ps[bk][:], scalar=gates[:, i, e_:e_ + 1],
                    in1=acc_t[:, fbk * 512:(fbk + 1) * 512], op0=ALU.mult, op1=ALU.add),
                     r=[f"ps{bk}", "gates", "acc_t"], w=["acc_t"])
            P.dma("sp", x3d[i * 128:(i + 1) * 128, :], acc_t, r=["acc_t"], w=[f"x3d{i}"], sem="accs")

    P.barrier()
    A.reset()
    xt = [A.view([128, D], F32) for _ in range(2)]
    ot = [A.view([128, D], F32) for _ in range(2)]
    P.dma("sp", gb[:], gf_d.partition_broadcast(128), w=["gb"], sem="gbl")
    outs = []
    for i in range(NT):
        b = i % 2
        P.dma("sp", xt[b], x3d[i * 128:(i + 1) * 128, :], r=[f"x3d{i}"], w=[f"xt{b}"], sem=f"xtl{b}")
        C.rstd(xt[b], f"xt{b}", D, ot[b], f"ot{b}", rs_t, "rs")
        P.op("dve", lambda e, b=b: e.scalar_tensor_tensor(out=ot[b], in0=xt[b], scalar=rs_t[:], in1=gb[:],
                                                          op0=ALU.mult, op1=ALU.mult),
             r=[f"xt{b}", "rs", "gb"], w=[f"ot{b}"])
        outs.append(P.dma("sp", out[i * 128:(i + 1) * 128, :], ot[b], r=[f"ot{b}"], sem=f"oto{b}"))
    nc = P.emit(final_wait_ops=outs)
    return C, nc


import numpy as np, ml_dtypes
def l1_inputs(z, b, hh, NH=8):
    w_in = z["mla_w_in"][0]
    w_in_ext = np.concatenate([w_in, w_in[:, 1056:1088], w_in[:, 1024:1056]], axis=1)
    wuq = z["mla_w_uq"][0].reshape(512, 16, 192)[:, hh * NH:(hh + 1) * NH]
    wuq_h = np.concatenate([wuq, wuq[:, :, 160:192], wuq[:, :, 128:160]], axis=2)
    wukv_h = z["mla_w_ukv"][0].reshape(512, 16, 256)[:, hh * NH:(hh + 1) * NH]
    inv = (10000.0 ** (-np.arange(0, 64, 2, dtype=np.float32) / 64)).astype(np.float32)
    invf = np.concatenate([inv, inv])[None].astype(np.float32)
    tri = np.triu(np.ones((128, 128), np.float32)).astype(ml_dtypes.bfloat16)
    pos = np.ascontiguousarray(z["positions"][b].reshape(-1, 128).T)
    return dict(x=z["x"][b], pos=pos, g=z["norm_mix"][0:1], w_in=np.ascontiguousarray(w_in_ext),
                qg=z["mla_q_norm"][0:1], kvg=z["mla_kv_norm"][0:1], w_uq=np.ascontiguousarray(wuq_h),
                w_ukv=np.ascontiguousarray(wukv_h), invf=invf, tri=tri)

def l3_inputs(z, x2b, hh):
    w = z["ssd_w_in"][0]
    zc = w[:, hh * 2048:(hh + 1) * 2048]
    xcw = w[:, 4096 + hh * 2048:4096 + (hh + 1) * 2048]
    Bc = w[:, 8192 + hh * 512:8192 + (hh + 1) * 512]
    Cc = w[:, 9216 + hh * 512:9216 + (hh + 1) * 512]
    dtc = w[:, 10240 + hh * 32:10240 + (hh + 1) * 32]
    w_cat = np.ascontiguousarray(np.concatenate([zc, xcw, Bc, Cc, dtc], axis=1))
    chans = np.concatenate([np.arange(hh * 2048, (hh + 1) * 2048), 4096 + np.arange(hh * 512, (hh + 1) * 512),
                            5120 + np.arange(hh * 512, (hh + 1) * 512)])
    cwc = z["ssd_conv_w"][0][:, chans]
    cw = np.ascontiguousarray(cwc.reshape(4, 24, 128).transpose(2, 1, 0))
    cb = np.ascontiguousarray(z["ssd_conv_b"][0][chans].reshape(24, 128).T)
    dtb = z["ssd_dt_bias"][0][hh * 32:(hh + 1) * 32][None]
    alog = z["ssd_a_log"][0][hh * 32:(hh + 1) * 32][None]
    dh = z["ssd_d"][0][hh * 32:(hh + 1) * 32]
    dcol = np.ascontiguousarray(np.repeat(dh, 64).reshape(16, 128).T)
    ng = np.ascontiguousarray(z["ssd_norm"][0][hh * 2048:(hh + 1) * 2048].reshape(16, 128).T)
    tri = np.triu(np.ones((128, 128), np.float32))
    ones = np.ones((128, 128), np.float32)
    return dict(x2=x2b, g=z["norm_mix"][1:2], w_cat=w_cat, cw=cw, cb=cb, dtb=np.ascontiguousarray(dtb),
                alog=np.ascontiguousarray(alog), dcol=dcol, ng=ng,
                c_tm=np.concatenate([tri, ones], 1), c_cm=np.concatenate([tri, ones, tri], 1))

def l4_consts():
    return dict(c_iota=np.tile(np.arange(896, dtype=np.float32), (128, 1)),
                c_tris=np.triu(np.ones((128, 128), np.float32), k=1).astype(ml_dtypes.bfloat16))


import numpy as np
import ml_dtypes
from concourse.bass_utils import run_bass_kernel_spmd

_PROGS = {}


def _prog(name, builder):
    if name not in _PROGS:
        _PROGS[name] = builder()
    return _PROGS[name]


def _run(name, builder, maps):
    C, nc = _prog(name, builder)
    cm = C.consts_np()
    full = []
    for m in maps:
        d = dict(m)
        d.update(cm)
        full.append(d)
    res = run_bass_kernel_spmd(nc, full, core_ids=list(range(8)))
    return res.results


def kernel(**inputs):
    z = {k: np.asarray(v) for k, v in inputs.items()}
    B, S = 4, 4096
    H = S // 2
    cores = [(c // 2, c % 2) for c in range(8)]
    r1 = _run("l1", build_l1, [l1_inputs(z, b, hh) for (b, hh) in cores])
    oT = {cores[c]: r1[c]["oT"] for c in range(8)}
    w_o, g0 = z["mla_w_o"][0], z["norm_ffn"][0:1]
    wg, wu, wd = z["ffn_w_gate"][0], z["ffn_w_up"][0], z["ffn_w_down"][0]
    maps = []
    for (b, half) in cores:
        oTc = np.ascontiguousarray(np.concatenate([oT[(b, 0)][:, half * H:(half + 1) * H],
                                                   oT[(b, 1)][:, half * H:(half + 1) * H]], axis=0))
        maps.append(dict(x=np.ascontiguousarray(z["x"][b, half * H:(half + 1) * H]), oT=oTc, w_o=w_o, g=g0,
                         wg=wg, wu=wu, wd=wd))
    r2 = _run("l2", build_l2, maps)
    x2 = np.empty((B, S, 2048), np.float32)
    for c, (b, half) in enumerate(cores):
        x2[b, half * H:(half + 1) * H] = r2[c]["x2"]
    r3 = _run("l3", build_l3, [l3_inputs(z, x2[b], hh) for (b, hh) in cores])
    yT = {cores[c]: r3[c]["yT"] for c in range(8)}
    l4c = l4_consts()
    maps = []
    for (b, half) in cores:
        yTc = np.ascontiguousarray(np.concatenate([yT[(b, 0)][:, half * H:(half + 1) * H],
                                                   yT[(b, 1)][:, half * H:(half + 1) * H]], axis=0))
        d = dict(x2=np.ascontiguousarray(x2[b, half * H:(half + 1) * H]), yT=yTc, w_o=z["ssd_w_o"][0],
                 g=z["norm_ffn"][1:2], gfin=z["norm_final"][None], router=z["moe_router"][0],
                 wg=z["moe_w_gate"][0], wu=z["moe_w_up"][0], wd=z["moe_w_down"][0])
        d.update(l4c)
        maps.append(d)
    r4 = _run("l4", build_l4, maps)
    out = np.empty((B, S, 2048), np.float32)
    for c, (b, half) in enumerate(cores):
        out[b, half * H:(half + 1) * H] = r4[c]["out"]
    return out
```
